# Optimizing a Trainium2 kernel written in Bass

```python
import math
import jax, jax.numpy as jnp
from jax import lax
import numpy as np

D_MODEL = 1024
BATCH = 8
SEQ = 4096
DEPTH = 4

GRID_W = 64
CTX_LEN = 256
N_MOD = 6
NORM_EPS = 1e-6

MLA_HEADS = 8
MLA_Q_RANK = 384
MLA_KV_RANK = 256
MLA_NOPE = 64
MLA_ROPE = 32
MLA_V = 64
ROPE_THETA = 10000.0
Q_BLOCK = 128

GDN_HEADS = 4
GDN_DK = 64
GDN_DV = 64
GDN_CONV = 3
GDN_CHUNK = 64

GLA_HEADS = 4
GLA_DK = 32
GLA_DV = 64
GLA_GATE_RANK = 16
GLA_NORMALIZER = 16.0
GLA_CHUNK = 16

FFN_HIDDEN = 2560
FFN_CONV = 3

D_MIX = MLA_HEADS * MLA_V + GDN_HEADS * GDN_DV + GLA_HEADS * GLA_DV
GDN_QKV = GDN_HEADS * (2 * GDN_DK + GDN_DV)
IN_SIZES = (MLA_Q_RANK, MLA_KV_RANK, MLA_ROPE,
            GDN_QKV, GDN_HEADS * GDN_DV, 2 * GDN_HEADS, 2 * GDN_HEADS,
            GLA_HEADS * GLA_DK, GLA_HEADS * GLA_DK, GLA_HEADS * GLA_DV, GLA_HEADS * GLA_DV,
            2 * GLA_GATE_RANK)
IN_SPLITS = tuple(int(s) for s in np.cumsum(IN_SIZES)[:-1])
D_IN_PROJ = sum(IN_SIZES)

kernel_name = 'hybrid_mla_gdn_gla_dit_block'

F32 = jnp.float32


def rmsnorm(x, g):
    xf = x.astype(F32)
    y = xf * lax.rsqrt(jnp.mean(xf * xf, axis=-1, keepdims=True) + NORM_EPS)
    return y.astype(x.dtype) * g


def l2norm(x):
    xf = x.astype(F32)
    return (xf * lax.rsqrt(jnp.sum(xf * xf, axis=-1, keepdims=True) + NORM_EPS)).astype(x.dtype)


def modulate(h, shift, scale):
    return h * (1 + scale) + shift


def dwconv_centred(x, w):
    K = w.shape[0]
    return lax.conv_general_dilated(
        x, w[:, None, :].astype(x.dtype), window_strides=(1,), padding=[(K // 2, K // 2)],
        dimension_numbers=('NWC', 'WIO', 'NWC'), feature_group_count=x.shape[-1])


def axial_angles(T):
    rows = T // GRID_W
    t = jnp.arange(rows * GRID_W)
    row = (t // GRID_W).astype(F32)
    col = (t % GRID_W).astype(F32)
    n = MLA_ROPE // 4
    inv = ROPE_THETA ** (-jnp.arange(n, dtype=F32) / n)
    ang = jnp.stack([row[:, None] * inv, col[:, None] * inv], axis=1)
    return jnp.cos(ang), jnp.sin(ang)


def apply_axial_rope(x, cos, sin):
    xr = x.reshape(*x.shape[:-1], 2, 2, MLA_ROPE // 4)
    x1, x2 = xr[..., 0, :], xr[..., 1, :]
    cos, sin = cos.astype(x.dtype), sin.astype(x.dtype)
    out = jnp.stack([x1 * cos - x2 * sin, x2 * cos + x1 * sin], axis=-2)
    return out.reshape(x.shape)


def mla_project(parts, q_norm, w_uq, kv_norm, w_ukv):
    cq, ckv, k_rope = parts[0], parts[1], parts[2]
    B, L = cq.shape[:2]
    q = (rmsnorm(cq, q_norm) @ w_uq).reshape(B, L, MLA_HEADS, MLA_NOPE + MLA_ROPE)
    kv = (rmsnorm(ckv, kv_norm) @ w_ukv).reshape(B, L, MLA_HEADS, MLA_NOPE + MLA_V)
    return q[..., :MLA_NOPE], q[..., MLA_NOPE:], kv[..., :MLA_NOPE], k_rope, kv[..., MLA_NOPE:]


def mla_attention(q_nope, q_rope, k_nope, k_rope, v):
    scale = (MLA_NOPE + MLA_ROPE) ** -0.5
    s = (jnp.einsum('bqhd,bkhd->bhqk', q_nope, k_nope)
         + jnp.einsum('bqhd,bkd->bhqk', q_rope, k_rope))
    p = jax.nn.softmax(s.astype(F32) * scale, axis=-1).astype(v.dtype)
    return jnp.einsum('bhqk,bkhd->bqhd', p, v)


def mla_attention_blocked(q_nope, q_rope, k_nope, k_rope, v):
    B, T = q_nope.shape[:2]
    nb = T // Q_BLOCK

    def blocks(a):
        return jnp.moveaxis(a.reshape(B, nb, Q_BLOCK, *a.shape[2:]), 1, 0)

    out = lax.map(lambda qs: mla_attention(qs[0], qs[1], k_nope, k_rope, v),
                  (blocks(q_nope), blocks(q_rope)))
    return jnp.moveaxis(out, 0, 1).reshape(B, T, *out.shape[3:])


def gated_delta_chunked(q, k, v, g, beta, S0):
    dt = v.dtype
    B, H, T, Dk = q.shape
    Dv = v.shape[-1]
    C = GDN_CHUNK
    N = T // C

    def f(a):
        return a.astype(F32).reshape(B, H, N, C, *a.shape[3:])

    q, k, v, g, beta = f(q) * Dk ** -0.5, f(k), f(v), f(g), f(beta)
    gc = jnp.cumsum(g, axis=-1)
    incl = jnp.tril(jnp.ones((C, C), bool))
    strict = jnp.tril(jnp.ones((C, C), bool), -1)
    diff = gc[..., :, None] - gc[..., None, :]
    decay = jnp.where(incl, jnp.exp(jnp.minimum(diff, 0.0)), 0.0)
    kb = k * beta[..., None]
    L = jnp.where(strict, jnp.einsum('bhnik,bhnjk->bhnij', kb, k) * decay, 0.0)
    Tinv = jnp.eye(C, dtype=F32) - L
    P = L
    for _ in range(int(math.log2(C)) - 1):
        P = P @ P
        Tinv = Tinv + Tinv @ P
    u = Tinv @ (v * beta[..., None])
    w = Tinv @ (kb * jnp.exp(gc)[..., None])
    A = jnp.einsum('bhnik,bhnjk->bhnij', q, k) * decay
    qd = q * jnp.exp(gc)[..., None]
    kend = k * jnp.exp(gc[..., -1:] - gc)[..., None]
    glast = jnp.exp(gc[..., -1])

    def step(S, xs):
        qd_, w_, u_, A_, kend_, glast_ = xs
        v_new = u_ - jnp.einsum('bhck,bhkv->bhcv', w_, S)
        o = jnp.einsum('bhck,bhkv->bhcv', qd_, S) + jnp.einsum('bhij,bhjv->bhiv', A_, v_new)
        S = S * glast_[..., None, None] + jnp.einsum('bhck,bhcv->bhkv', kend_, v_new)
        return S, o

    xs = tuple(jnp.moveaxis(a, 2, 0) for a in (qd, w, u, A, kend, glast))
    S, o = lax.scan(step, S0.astype(F32), xs)
    o = jnp.moveaxis(o, 0, 2).reshape(B, H, T, Dv)
    return o.astype(dt), S


def gla_chunked(q, k, v, la, S0):
    dt = v.dtype
    B, H, T, Dk = q.shape
    Dv = v.shape[-1]
    C = GLA_CHUNK
    N = T // C

    def f(a):
        return a.astype(F32).reshape(B, H, N, C, *a.shape[3:])

    q, k, v, la = f(q) * Dk ** -0.5, f(k), f(v), f(la)
    b = jnp.cumsum(la, axis=3)
    incl = jnp.tril(jnp.ones((C, C), bool))[:, :, None]
    diff = b[:, :, :, :, None, :] - b[:, :, :, None, :, :]
    decay = jnp.where(incl, jnp.exp(jnp.minimum(diff, 0.0)), 0.0)
    A = jnp.einsum('bhnik,bhnjk,bhnijk->bhnij', q, k, decay)
    o_intra = jnp.einsum('bhnij,bhnjv->bhniv', A, v)
    qd = q * jnp.exp(b)
    kend = k * jnp.exp(b[..., -1:, :] - b)
    glast = jnp.exp(b[..., -1, :])

    def step(S, xs):
        qd_, kend_, v_, glast_ = xs
        o = jnp.einsum('bhck,bhkv->bhcv', qd_, S)
        S = S * glast_[..., :, None] + jnp.einsum('bhck,bhcv->bhkv', kend_, v_)
        return S, o

    xs = tuple(jnp.moveaxis(a, 2, 0) for a in (qd, kend, v, glast))
    S, o_inter = lax.scan(step, S0.astype(F32), xs)
    o = o_intra + jnp.moveaxis(o_inter, 0, 2)
    return o.reshape(B, H, T, Dv).astype(dt), S


def rev(a, d):
    return jnp.flip(a, axis=2) if d else a


def bidirectional_scan(chunk_fn, seq_c, dir_c, seq_x, dir_x, state_shape):
    outs_c, outs_x = [], []
    for d in range(2):
        S0 = jnp.zeros(state_shape, F32)
        o_c, S_c = chunk_fn(*[rev(a, d) for a in seq_c], *[rev(a[d], d) for a in dir_c], S0)
        o_x, _ = chunk_fn(*[rev(a, d) for a in seq_x], *[rev(a[d], d) for a in dir_x], S_c)
        outs_c.append(rev(o_c, d))
        outs_x.append(rev(o_x, d))
    return outs_c[0] + outs_c[1], outs_x[0] + outs_x[1]


def gdn_features(parts, conv_w, a_log, dt_bias):
    qkv = jax.nn.silu(dwconv_centred(parts[3], conv_w))
    B, L = qkv.shape[:2]
    q, k, v = jnp.split(qkv, [GDN_HEADS * GDN_DK, 2 * GDN_HEADS * GDN_DK], axis=-1)

    def heads(a, d):
        return jnp.moveaxis(a.reshape(B, L, GDN_HEADS, d), 2, 1)

    q, k, v = l2norm(heads(q, GDN_DK)), l2norm(heads(k, GDN_DK)), heads(v, GDN_DV)
    a = parts[5].reshape(B, L, 2, GDN_HEADS).astype(F32)
    bb = parts[6].reshape(B, L, 2, GDN_HEADS).astype(F32)
    g = -jnp.exp(a_log.astype(F32)) * jax.nn.softplus(a + dt_bias.astype(F32))
    beta = jax.nn.sigmoid(bb)
    g = jnp.transpose(g, (2, 0, 3, 1))
    beta = jnp.transpose(beta, (2, 0, 3, 1))
    return (q, k, v), (g, beta), parts[4]


def gla_features(parts, w_gk, b_gk):
    B, L = parts[7].shape[:2]

    def heads(a, d):
        return jnp.moveaxis(a.reshape(B, L, GLA_HEADS, d), 2, 1)

    q, k, v = heads(parts[7], GLA_DK), heads(parts[8], GLA_DK), heads(parts[9], GLA_DV)
    lr = parts[11].reshape(B, L, 2, GLA_GATE_RANK)
    gk = jnp.einsum('bldr,drk->dblk', lr, w_gk) + b_gk[:, None, None, :]
    la = jax.nn.log_sigmoid(gk.astype(F32)) / GLA_NORMALIZER
    la = jnp.moveaxis(la.reshape(2, B, L, GLA_HEADS, GLA_DK), 3, 2)
    return (q, k, v), (la,), parts[10]


def merge_heads(mla_o, gdn_o, gdn_z, gdn_norm, gla_o, gla_g, gla_norm):
    B, L = mla_o.shape[:2]
    gdn_o = rmsnorm(jnp.moveaxis(gdn_o, 1, 2), gdn_norm) * jax.nn.silu(gdn_z.reshape(B, L, GDN_HEADS, GDN_DV))
    gla_o = rmsnorm(jnp.moveaxis(gla_o, 1, 2), gla_norm) * jax.nn.silu(gla_g.reshape(B, L, GLA_HEADS, GLA_DV))
    return jnp.concatenate([mla_o.reshape(B, L, -1), gdn_o.reshape(B, L, -1), gla_o.reshape(B, L, -1)], axis=-1)


def token_mixing(hc, hx, w_in, mla_q_norm, mla_w_uq, mla_kv_norm, mla_w_ukv,
                 gdn_conv_w, gdn_a_log, gdn_dt_bias, gdn_norm,
                 gla_w_gk, gla_b_gk, gla_norm, w_out, need_ctx_out):
    B, T, _ = hx.shape
    pc = jnp.split(hc @ w_in, IN_SPLITS, axis=-1)
    px = jnp.split(hx @ w_in, IN_SPLITS, axis=-1)

    qn_c, qr_c, kn_c, kr_c, v_c = mla_project(pc, mla_q_norm, mla_w_uq, mla_kv_norm, mla_w_ukv)
    qn_x, qr_x, kn_x, kr_x, v_x = mla_project(px, mla_q_norm, mla_w_uq, mla_kv_norm, mla_w_ukv)
    cos, sin = axial_angles(T)
    qr_x = apply_axial_rope(qr_x, cos[:, None], sin[:, None])
    kr_x = apply_axial_rope(kr_x, cos, sin)

    def cat(a, b):
        return jnp.concatenate([a, b], axis=1)

    mla_x = mla_attention_blocked(qn_x, qr_x, cat(kn_c, kn_x), cat(kr_c, kr_x), cat(v_c, v_x))

    seq_c, dir_c, z_c = gdn_features(pc, gdn_conv_w, gdn_a_log, gdn_dt_bias)
    seq_x, dir_x, z_x = gdn_features(px, gdn_conv_w, gdn_a_log, gdn_dt_bias)
    gdn_c, gdn_x = bidirectional_scan(gated_delta_chunked, seq_c, dir_c, seq_x, dir_x,
                                      (B, GDN_HEADS, GDN_DK, GDN_DV))

    sq_c, dr_c, gg_c = gla_features(pc, gla_w_gk, gla_b_gk)
    sq_x, dr_x, gg_x = gla_features(px, gla_w_gk, gla_b_gk)
    gla_c, gla_x = bidirectional_scan(gla_chunked, sq_c, dr_c, sq_x, dr_x,
                                      (B, GLA_HEADS, GLA_DK, GLA_DV))

    y_x = merge_heads(mla_x, gdn_x, z_x, gdn_norm, gla_x, gg_x, gla_norm) @ w_out
    y_c = None
    if need_ctx_out:
        mla_c = mla_attention(qn_c, qr_c, kn_c, kr_c, v_c)
        y_c = merge_heads(mla_c, gdn_c, z_c, gdn_norm, gla_c, gg_c, gla_norm) @ w_out
    return y_c, y_x


def conv_ffn(h, w_in, b_in, conv_w, conv_b, w_out):
    z = dwconv_centred(h @ w_in + b_in, conv_w) + conv_b
    a, gt = jnp.split(z, 2, axis=-1)
    return (a * jax.nn.silu(gt)) @ w_out


def setup_inputs(seed: int = 0) -> dict:
    key = jax.random.key(seed)
    ks = jax.random.split(key, 28)
    L = DEPTH

    def nrm(k, shape, scale):
        return jax.random.normal(k, shape, F32) * scale

    def gain(k, shape):
        return 1.0 + 0.05 * jax.random.normal(k, shape, F32)

    dt0 = jax.random.uniform(ks[17], (L, 2, GDN_HEADS), F32, 0.001, 0.1)
    return {
        'x': nrm(ks[0], (BATCH, SEQ, D_MODEL), 1.0),
        'c': nrm(ks[1], (BATCH, D_MODEL), 1.0),
        'ctx': nrm(ks[2], (BATCH, CTX_LEN, D_MODEL), 1.0),
        'c_ctx': nrm(ks[3], (D_MODEL,), 1.0),
        'w_ada': nrm(ks[4], (L, D_MODEL, N_MOD * D_MODEL), 0.5 * D_MODEL ** -0.5),
        'b_ada': nrm(ks[5], (L, N_MOD * D_MODEL), 0.02),
        'norm_mix_pre': gain(ks[6], (L, D_MODEL)),
        'norm_mix_post': gain(ks[7], (L, D_MODEL)),
        'norm_ffn_pre': gain(ks[8], (L, D_MODEL)),
        'norm_ffn_post': gain(ks[9], (L, D_MODEL)),
        'w_in': nrm(ks[10], (L, D_MODEL, D_IN_PROJ), D_MODEL ** -0.5),
        'mla_q_norm': gain(ks[11], (L, MLA_Q_RANK)),
        'mla_w_uq': nrm(ks[12], (L, MLA_Q_RANK, MLA_HEADS * (MLA_NOPE + MLA_ROPE)), MLA_Q_RANK ** -0.5),
        'mla_kv_norm': gain(ks[13], (L, MLA_KV_RANK)),
        'mla_w_ukv': nrm(ks[14], (L, MLA_KV_RANK, MLA_HEADS * (MLA_NOPE + MLA_V)), MLA_KV_RANK ** -0.5),
        'gdn_conv_w': nrm(ks[15], (L, GDN_CONV, GDN_QKV), GDN_CONV ** -0.5),
        'gdn_a_log': jnp.log(jax.random.uniform(ks[16], (L, 2, GDN_HEADS), F32, 1.0, 16.0)),
        'gdn_dt_bias': jnp.log(jnp.expm1(dt0)),
        'gdn_norm': gain(ks[18], (L, GDN_DV)),
        'gla_w_gk': nrm(ks[19], (L, 2, GLA_GATE_RANK, GLA_HEADS * GLA_DK), GLA_GATE_RANK ** -0.5),
        'gla_b_gk': nrm(ks[20], (L, 2, GLA_HEADS * GLA_DK), 0.1),
        'gla_norm': gain(ks[21], (L, GLA_DV)),
        'w_out': nrm(ks[22], (L, D_MIX, D_MODEL), D_MIX ** -0.5),
        'ffn_w_in': nrm(ks[23], (L, D_MODEL, 2 * FFN_HIDDEN), D_MODEL ** -0.5),
        'ffn_b_in': nrm(ks[24], (L, 2 * FFN_HIDDEN), 0.02),
        'ffn_conv_w': nrm(ks[25], (L, FFN_CONV, 2 * FFN_HIDDEN), FFN_CONV ** -0.5),
        'ffn_conv_b': nrm(ks[26], (L, 2 * FFN_HIDDEN), 0.02),
        'ffn_w_out': nrm(ks[27], (L, FFN_HIDDEN, D_MODEL), FFN_HIDDEN ** -0.5),
    }


def reference(x, c, ctx, c_ctx, w_ada, b_ada, norm_mix_pre, norm_mix_post, norm_ffn_pre, norm_ffn_post,
              w_in, mla_q_norm, mla_w_uq, mla_kv_norm, mla_w_ukv, gdn_conv_w, gdn_a_log, gdn_dt_bias,
              gdn_norm, gla_w_gk, gla_b_gk, gla_norm, w_out, ffn_w_in, ffn_b_in, ffn_conv_w, ffn_conv_b,
              ffn_w_out):
    xc = ctx
    for i in range(DEPTH):
        last = i == DEPTH - 1
        mod_x = [m[:, None, :] for m in jnp.split(jax.nn.silu(c) @ w_ada[i] + b_ada[i], N_MOD, axis=-1)]
        mod_c = jnp.split(jax.nn.silu(c_ctx) @ w_ada[i] + b_ada[i], N_MOD, axis=-1)

        hx = modulate(rmsnorm(x, norm_mix_pre[i]), mod_x[0], mod_x[1])
        hc = modulate(rmsnorm(xc, norm_mix_pre[i]), mod_c[0], mod_c[1])
        y_c, y_x = token_mixing(hc, hx, w_in[i], mla_q_norm[i], mla_w_uq[i], mla_kv_norm[i], mla_w_ukv[i],
                                gdn_conv_w[i], gdn_a_log[i], gdn_dt_bias[i], gdn_norm[i],
                                gla_w_gk[i], gla_b_gk[i], gla_norm[i], w_out[i], not last)
        x = x + mod_x[2] * rmsnorm(y_x, norm_mix_post[i])

        hx = modulate(rmsnorm(x, norm_ffn_pre[i]), mod_x[3], mod_x[4])
        x = x + mod_x[5] * rmsnorm(conv_ffn(hx, ffn_w_in[i], ffn_b_in[i], ffn_conv_w[i], ffn_conv_b[i],
                                            ffn_w_out[i]), norm_ffn_post[i])

        if not last:
            xc = xc + mod_c[2] * rmsnorm(y_c, norm_mix_post[i])
            hc = modulate(rmsnorm(xc, norm_ffn_pre[i]), mod_c[3], mod_c[4])
            xc = xc + mod_c[5] * rmsnorm(conv_ffn(hc, ffn_w_in[i], ffn_b_in[i], ffn_conv_w[i], ffn_conv_b[i],
                                                  ffn_w_out[i]), norm_ffn_post[i])
    return x
```

```python
import contextlib
import numpy as np
import ml_dtypes
import concourse.bass as bass
import concourse.mybir as mybir
from concourse.bass_utils import run_bass_kernel_spmd

F32 = mybir.dt.float32
BF16 = mybir.dt.bfloat16
AF = mybir.ActivationFunctionType
ALU = mybir.AluOpType
AX = mybir.AxisListType

NL = 4
D = 1024
TC = 256
TX = 4096
TT = TC + TX
NT = TT // 128
SEGS = ((0, TC), (TC, TX))
EPS = 1e-6
BIG = 30000.0
NW1 = 2832
NV = 303
NB = 400
V_BADA, V_NMP, V_NMO, V_NFP, V_NFO, V_FBI, V_FCW, V_FCB, V_GCW, V_QN, V_KVN = (
    0, 48, 56, 64, 72, 80, 120, 240, 280, 298, 301)
B_DTB, B_ALOG, B_GDNN, B_GLAN, B_BGK = 0, 8, 16, 80, 144

MAXV = 30000
GDN_CUT = 0
GDN_VAR = 0
GDN_TILES = None
NPOOL = 24


class Op:
    __slots__ = ("eng", "fn", "deps", "need", "tok", "isdma")

    def __init__(self, eng, fn, isdma):
        self.eng = eng
        self.fn = fn
        self.deps = []
        self.need = False
        self.tok = None
        self.isdma = isdma


class Prog:
    ENGS = ("pe", "act", "dve", "pool", "sp")

    def __init__(self, nc):
        self.nc = nc
        self.ops = {e: [] for e in self.ENGS}
        self.lastw = {}
        self.readers = {}
        self.dmas = []
        self.nsem = 0
        self.enabled = True

    def op(self, eng, fn, reads=(), writes=(), dma=False):
        o = Op(eng, fn, dma)
        if not self.enabled:
            return o
        reads = _keys(reads)
        writes = _keys(writes)
        deps = set()
        for k in reads:
            w = self.lastw.get(k)
            if w is not None:
                deps.add(w)
        for k in writes:
            w = self.lastw.get(k)
            if w is not None:
                deps.add(w)
            rd = self.readers.get(k)
            if rd:
                for r in rd.values():
                    if isinstance(r, list):
                        deps.update(r)
                    else:
                        deps.add(r)
        for d in deps:
            if (not dma) and eng == "pe" and d.eng == "pe" and not d.isdma:
                continue
            d.need = True
            o.deps.append(d)
        if dma:
            o.need = True
            self.dmas.append(o)
        for k in writes:
            self.lastw[k] = o
            self.readers[k] = {}
        for k in reads:
            rd = self.readers.setdefault(k, {})
            if dma:
                rd.setdefault("dma", []).append(o)
            else:
                rd[eng] = o
        self.ops[eng].append(o)
        return o

    def barrier(self):
        if not self.enabled:
            return
        lasts = []
        for e in self.ENGS:
            for o in reversed(self.ops[e]):
                if not o.isdma:
                    lasts.append(o)
                    break
        dm = self.dmas
        self.dmas = []
        for e in self.ENGS:
            o = Op(e, lambda en: en.nop(), False)
            for d in lasts + dm:
                if d.eng == e and not d.isdma:
                    continue
                d.need = True
                o.deps.append(d)
            o.need = True
            self.ops[e].append(o)
        self.lastw = {}
        self.readers = {}

    def mm(self, out, lhsT, rhs, start=True, stop=True, reads=(), writes=(), **kw):
        return self.op("pe", lambda e: e.matmul(out, lhsT, rhs, start=start, stop=stop, **kw),
                       reads, writes)

    def tr(self, out, in_, ident, reads=(), writes=()):
        return self.op("pe", lambda e: e.transpose(out, in_, ident), reads, writes)

    def act(self, out, in_, func, reads=(), writes=(), **kw):
        return self.op("act", lambda e: e.activation(out, in_, func, **kw), reads, writes)

    def dma(self, out, in_, reads=(), writes=(), q="sp", **kw):
        return self.op(q, lambda e: e.dma_start(out=out, in_=in_, **kw), reads, writes, dma=True)

    def v(self, name, *args, reads=(), writes=(), eng="dve", **kw):
        return self.op(eng, lambda e: getattr(e, name)(*args, **kw), reads, writes)

    def finalize(self, stack):
        nc = self.nc
        for eng in self.ENGS:
            cnt = 0
            cur = None
            for o in self.ops[eng]:
                if o.isdma or not o.need:
                    continue
                if cur is None or cnt >= MAXV:
                    cur = stack.enter_context(nc.semaphore(f"s_{eng}_{self.nsem}"))
                    self.nsem += 1
                    cnt = 0
                cnt += 1
                o.tok = (cur, cnt)
        for eng in self.ENGS:
            pool = None
            n = 0
            for o in self.ops[eng]:
                if not o.isdma:
                    continue
                if pool is None:
                    pool = [stack.enter_context(nc.semaphore(f"d_{eng}_{i}")) for i in range(NPOOL)]
                    vals = [0] * NPOOL
                    last = [None] * NPOOL
                i = n % NPOOL
                n += 1
                if last[i] is not None:
                    o.deps.append(last[i])
                vals[i] += 16
                o.tok = (pool[i], vals[i])
                last[i] = o
        with nc.Block() as block:
            def body(eng):
                def f(e):
                    waited = {}
                    for o in self.ops[eng]:
                        need = {}
                        for d in o.deps:
                            s, val = d.tok
                            if waited.get(s.num, 0) >= val:
                                continue
                            if need.get(s.num, (None, 0))[1] < val:
                                need[s.num] = (s, val)
                        for s, val in need.values():
                            e.wait_ge(s, val)
                            waited[s.num] = val
                        ins = o.fn(e)
                        if o.need:
                            s, val = o.tok
                            ins.then_inc(s, 16 if o.isdma else 1)
                return f
            block.tensor(body("pe"))
            block.scalar(body("act"))
            block.vector(body("dve"))
            block.gpsimd(body("pool"))
            block.sync(body("sp"))


class Tl:
    __slots__ = ("ap", "key")

    def __init__(self, ap, key):
        self.ap = ap
        self.key = key

    def __getitem__(self, idx):
        return self.ap[idx]


def _keys(items):
    return [getattr(k, "key", k) for k in items]


class Arena:
    def __init__(self, t, nwords):
        self.t = t
        self.n = nwords
        self.off = 0
        self.cnt = 0

    def reset(self):
        self.off = 0

    def alloc(self, shape, dt=F32, name=None):
        free = int(np.prod(shape[1:]))
        nw = free if dt == F32 else (free + 1) // 2
        assert self.off + nw <= self.n, f"arena overflow {self.off + nw} > {self.n} ({name})"
        a = self.t[0:shape[0], self.off:self.off + nw]
        self.off += nw
        if dt != F32:
            a = a.bitcast(dt)[:, 0:free]
        if len(shape) == 3:
            a = a.rearrange("p (a b) -> p a b", a=shape[1], b=shape[2])
        elif len(shape) == 4:
            a = a.rearrange("p (a b c) -> p a b c", a=shape[1], b=shape[2], c=shape[3])
        self.cnt += 1
        return Tl(a, f"ar{self.cnt}_{name}")


def _chunkT(v):
    return np.ascontiguousarray(v.reshape(-1, 128).T)


def _rope_swap_idx():
    idx = np.arange(32).reshape(2, 2, 8)
    return idx[:, ::-1, :].reshape(32)


def host_consts():
    c = {}
    j = np.arange(128)[:, None]
    i = np.arange(128)[None, :]
    ones = np.ones((128, 128), np.float32)
    ident = np.eye(128, dtype=np.float32)
    tri0 = (j <= i).astype(np.float32)
    tri1 = (j >= i).astype(np.float32)
    mk0 = (i >= j).astype(np.float32)
    mk1 = (i <= j).astype(np.float32)
    blk = (np.arange(128)[:, None] // 32 == np.arange(4)[None, :]).astype(np.float32)
    blkv = np.repeat(blk, 64, axis=1)
    erope = np.zeros((128, 96), np.float32)
    erope[np.arange(32), 64 + np.arange(32)] = 1.0
    nm = np.zeros((128, 2, 2, 4, 128), np.float32)
    nm[:, 0, 0] = np.where(i >= j, 0.0, -BIG)[:, None, :]
    nm[:, 0, 1] = np.where(i > j, 0.0, -BIG)[:, None, :]
    nm[:, 1, 0] = np.where(i <= j, 0.0, -BIG)[:, None, :]
    nm[:, 1, 1] = np.where(i < j, 0.0, -BIG)[:, None, :]
    bd = ((j // 64) == (i // 64)).astype(np.float32)
    c["cf32"] = np.ascontiguousarray(np.concatenate(
        [ones, ident, tri0, tri1, mk0, mk1, blk, blkv, erope, nm.reshape(128, 2048), bd], axis=1))
    t = np.arange(TX)
    row = (t // 64).astype(np.float32)
    col = (t % 64).astype(np.float32)
    inv = (np.float32(10000.0) ** (-np.arange(8, dtype=np.float32) / np.float32(8))).astype(np.float32)
    ang = np.stack([row[:, None] * inv, col[:, None] * inv], axis=1).astype(np.float32)
    cos = np.cos(ang).astype(np.float32)
    sin = np.sin(ang).astype(np.float32)
    C = np.stack([cos, cos], axis=2)
    S = np.stack([-sin, sin], axis=2)
    rope = np.stack([C.reshape(TX, 32).T, S.reshape(TX, 32).T], axis=0)
    c["rope"] = np.ascontiguousarray(rope.astype(np.float32))
    return c


C_ONES, C_ID, C_TRI0, C_TRI1, C_MK0, C_MK1, C_BLK, C_BLKV, C_EROPE, C_NM = (
    0, 128, 256, 384, 512, 640, 768, 772, 1028, 1124)
C_BD = 1124 + 2048
NCF = 1124 + 2048 + 128


def host_weights(inp):
    w = {}
    L = NL
    vec = np.zeros((L, 128, NV), np.float32)
    bcv = np.zeros((L, 128, NB), np.float32)
    w1 = np.zeros((L, D, NW1), np.float32)
    wuq = np.zeros((L, 384, 8, 128), np.float32)
    wukvk = np.zeros((L, 256, 8, 96), np.float32)
    wukvv = np.zeros((L, 256, 8, 64), np.float32)
    wgk = np.zeros((L, 64, 128), np.float32)
    sw = _rope_swap_idx()
    for l in range(L):
        vec[l, :, V_BADA:V_BADA + 48] = _chunkT(inp["b_ada"][l])
        vec[l, :, V_NMP:V_NMP + 8] = _chunkT(inp["norm_mix_pre"][l])
        vec[l, :, V_NMO:V_NMO + 8] = _chunkT(inp["norm_mix_post"][l])
        vec[l, :, V_NFP:V_NFP + 8] = _chunkT(inp["norm_ffn_pre"][l])
        vec[l, :, V_NFO:V_NFO + 8] = _chunkT(inp["norm_ffn_post"][l])
        vec[l, :, V_FBI:V_FBI + 40] = _chunkT(inp["ffn_b_in"][l])
        for t in range(3):
            vec[l, :, V_FCW + 40 * t:V_FCW + 40 * (t + 1)] = _chunkT(inp["ffn_conv_w"][l, t])
            vec[l, :, V_GCW + 6 * t:V_GCW + 6 * (t + 1)] = _chunkT(inp["gdn_conv_w"][l, t])
        vec[l, :, V_FCB:V_FCB + 40] = _chunkT(inp["ffn_conv_b"][l])
        vec[l, :, V_QN:V_QN + 3] = _chunkT(inp["mla_q_norm"][l])
        vec[l, :, V_KVN:V_KVN + 2] = _chunkT(inp["mla_kv_norm"][l])
        bcv[l, :, B_DTB:B_DTB + 8] = inp["gdn_dt_bias"][l].reshape(8)[None, :]
        bcv[l, :, B_ALOG:B_ALOG + 8] = inp["gdn_a_log"][l].reshape(8)[None, :]
        bcv[l, :, B_GDNN:B_GDNN + 64] = inp["gdn_norm"][l][None, :]
        bcv[l, :, B_GLAN:B_GLAN + 64] = inp["gla_norm"][l][None, :]
        bcv[l, :, B_BGK:B_BGK + 256] = inp["gla_b_gk"][l].reshape(256)[None, :]
        wi = inp["w_in"][l]
        o_cq, o_ckv, o_kr, o_gqkv, o_gz, o_ga, o_gb, o_lq, o_lk, o_lv, o_lg, o_lr = (
            0, 384, 640, 672, 1440, 1696, 1704, 1712, 1840, 1968, 2224, 2480)
        w1[l, :, 0:384] = wi[:, o_cq:o_cq + 384]
        w1[l, :, 384:640] = wi[:, o_ckv:o_ckv + 256]
        w1[l, :, 640:672] = wi[:, o_kr:o_kr + 32]
        w1[l, :, 672:704] = wi[:, o_kr:o_kr + 32][:, sw]
        w1[l, :, 704:1472] = wi[:, o_gqkv:o_gqkv + 768]
        w1[l, :, 1472:1600] = wi[:, o_lq:o_lq + 128]
        w1[l, :, 1600:1728] = wi[:, o_lk:o_lk + 128]
        w1[l, :, 1728:1744] = wi[:, o_lr:o_lr + 16]
        w1[l, :, 1760:1776] = wi[:, o_lr + 16:o_lr + 32]
        w1[l, :, 1792:2048] = wi[:, o_gz:o_gz + 256]
        w1[l, :, 2048:2176] = wi[:, o_lq:o_lq + 128]
        w1[l, :, 2176:2304] = wi[:, o_lk:o_lk + 128]
        w1[l, :, 2304:2560] = wi[:, o_lv:o_lv + 256]
        w1[l, :, 2560:2816] = wi[:, o_lg:o_lg + 256]
        w1[l, :, 2816:2824] = wi[:, o_ga:o_ga + 8]
        w1[l, :, 2824:2832] = wi[:, o_gb:o_gb + 8]
        uq = inp["mla_w_uq"][l].reshape(384, 8, 96)
        wuq[l, :, :, 0:96] = uq
        wuq[l, :, :, 96:128] = uq[:, :, 64:96][:, :, sw]
        ukv = inp["mla_w_ukv"][l].reshape(256, 8, 128)
        wukvk[l, :, :, 0:64] = ukv[:, :, 0:64]
        wukvv[l] = ukv[:, :, 64:128]
        wgk[l, 0:16] = inp["gla_w_gk"][l, 0]
        wgk[l, 32:48] = inp["gla_w_gk"][l, 1]
    w["vec"] = vec
    w["bcv"] = bcv
    w["w1"] = w1
    w["wuq"] = wuq.reshape(L, 384, 1024)
    w["wukvk"] = wukvk.reshape(L, 256, 768)
    w["wukvv"] = wukvv.reshape(L, 256, 512)
    w["wgk"] = wgk
    w["w_ada"] = np.ascontiguousarray(inp["w_ada"])
    w["wout"] = np.ascontiguousarray(inp["w_out"])
    w["fw1"] = np.ascontiguousarray(inp["ffn_w_in"])
    w["fw2"] = np.ascontiguousarray(inp["ffn_w_out"])
    return w


AR_WORDS = 45000


def build(nl=NL, dbg=(), stop_after=None, phases=None):
    NLW = nl
    nc = bass.Bass("TRN2", target_bir_lowering=False)
    st = contextlib.ExitStack()
    P = Prog(nc)

    def din(name, shape, dt=F32):
        return nc.dram_tensor(name, list(shape), dt, kind="ExternalInput").ap()

    def dscr(name, shape, dt=F32):
        kind = "ExternalOutput" if name in dbg else "Internal"
        return nc.dram_tensor(name, list(shape), dt, kind=kind).ap()

    xin = din("xin", [D, TT])
    cc = din("cc", [D, 2])
    w_ada = din("w_ada", [NLW, D, 6 * D])
    vec_d = din("vec", [NLW, 128, NV])
    bcv_d = din("bcv", [NLW, 128, NB])
    w1_d = din("w1", [NLW, D, NW1])
    wuq_d = din("wuq", [NLW, 384, 1024])
    wukvk_d = din("wukvk", [NLW, 256, 768])
    wukvv_d = din("wukvv", [NLW, 256, 512])
    wgk_d = din("wgk", [NLW, 64, 128])
    wout_d = din("wout", [NLW, D, D])
    fw1_d = din("fw1", [NLW, D, 5120])
    fw2_d = din("fw2", [NLW, 2560, D])
    cf32_d = din("cf32", [128, NCF])
    rope_d = din("rope", [2, 32, TX])
    out_d = nc.dram_tensor("out", [D, TX], F32, kind="ExternalOutput").ap()

    XT = dscr("XT", [D, TT])
    QT = dscr("QT", [8, 96, TT], BF16)
    KT = dscr("KT", [8, 96, TT], BF16)
    VT = dscr("VT", [TT, 8 * 65], BF16)
    GQKV = dscr("GQKV", [768, TT], BF16)
    LQKT = dscr("LQKT", [256, TT], BF16)
    LRT = dscr("LRT", [64, TT], F32)
    TMB = dscr("TMB", [TT, 1024], BF16)
    TMF = dscr("TMF", [TT, 16], F32)
    MT = dscr("MT", [D, TT], BF16)
    XM = dscr("XM", [D, TT])
    GN = dscr("GN", [TT, 768], BF16)
    GNT = dscr("GNT", [NT, 128, 512], BF16)
    GG = dscr("GG", [TT, 24], F32)
    GO = dscr("GO", [2, TT, 256], F32)
    LO = dscr("LO", [2, TT, 256], F32)

    def sb(name, shape, dt=F32):
        return Tl(st.enter_context(nc.sbuf_tensor(name, list(shape), dt)), name)

    cf = sb("cf", [128, NCF])
    identb = sb("identb", [128, 128], BF16)
    eropeb = sb("eropeb", [32, 96], BF16)
    vec = sb("vecs", [128, NLW, NV])
    bcv = sb("bcvs", [128, NLW, NB])
    mods = sb("mods", [128, NL, 48, 2])
    drv = sb("drv", [128, 4, 8, 2])
    epsb = sb("epsb", [128, 1])
    oneb = sb("oneb", [128, 1])
    arena_t = st.enter_context(nc.sbuf_tensor("arena", [128, AR_WORDS], F32))
    AR = Arena(arena_t, AR_WORDS)
    pst = st.enter_context(nc.psum_tensor("pst", [128, 8, 512], F32))
    PSK = [f"ps{b}" for b in range(8)]
    psn = [0]

    def nps():
        b = psn[0] % 8
        psn[0] += 1
        return Tl(pst[:, b, :], PSK[b])

    dbgouts = {}

    def dump(name, tl, shape, dt=F32):
        if name not in dbg:
            return
        d = nc.dram_tensor(name, list(shape), dt, kind="ExternalOutput").ap()
        P.dma(d, tl[:], reads=[tl], q="pool")

    def nps2():
        if psn[0] % 2:
            psn[0] += 1
        b = psn[0] % 8
        psn[0] += 2
        return b

    class PSA:
        def __init__(self, banks):
            self.banks = banks
            self.n = 0

        def nps(self):
            b = self.banks[self.n % len(self.banks)]
            self.n += 1
            return Tl(pst[:, b, :], PSK[b])

        def nps2(self):
            if self.n % 2:
                self.n += 1
            b = self.banks[self.n % len(self.banks)]
            self.n += 2
            return b

    def pv2(b, x):
        return pst[:, b:b + 2, 0:2 * x].rearrange("p q (c x) -> p q c x", c=2)

    ONES = cf[:, C_ONES:C_ONES + 128]
    IDENT = cf[:, C_ID:C_ID + 128]

    P.dma(cf[:], cf32_d, writes=[cf])
    P.dma(vec[:], vec_d.rearrange("l p n -> p l n"), writes=[vec])
    P.dma(bcv[:], bcv_d.rearrange("l p n -> p l n"), writes=[bcv])
    P.v("tensor_copy", identb[:], IDENT, reads=[cf], writes=[identb])
    P.v("tensor_copy", eropeb[:], cf[0:32, C_EROPE:C_EROPE + 96], reads=[cf], writes=[eropeb])
    P.v("memset", epsb[:], EPS, writes=[epsb])
    P.v("memset", oneb[:], 1.0, writes=[oneb])
    for k in range(8):
        P.dma(XT[k * 128:(k + 1) * 128, :], xin[k * 128:(k + 1) * 128, :], q="pool")

    def ph(name):
        P.enabled = (phases is None or name in phases)

    ph("0")
    AR.reset()
    cs = AR.alloc([128, 8, 2], F32, "cs")
    wst = [AR.alloc([128, 8, 512], F32, f"wada{i}") for i in range(2)]
    P.dma(cs[:], cc.rearrange("(k p) s -> p k s", p=128), writes=[cs])
    P.act(cs[:], cs[:], AF.Silu, reads=[cs], writes=[cs])
    gi = 0
    for l in range(nl):
        pm = nps()
        for g in range(12):
            ws = wst[gi % 2]
            gi += 1
            P.dma(ws[:], w_ada[l, :, g * 512:(g + 1) * 512].rearrange("(k p) n -> p k n", p=128), writes=[ws])
            for j in range(4):
                c = g * 4 + j
                for k in range(8):
                    P.mm(pm[:, 2 * c:2 * c + 2], ws[:, k, j * 128:(j + 1) * 128], cs[:, k, :],
                         start=(k == 0), stop=(k == 7), reads=[ws, cs], writes=[pm])
        P.v("tensor_tensor", mods[:, l], pm[:, 0:96].rearrange("p (c s) -> p c s", s=2),
            vec[:, l, V_BADA:V_BADA + 48].unsqueeze(2).to_broadcast([128, 48, 2]), ALU.add,
            reads=[pm, vec], writes=[mods])
    dump("d_mods", mods, [128, NL, 48, 2])
    P.barrier()
    P.enabled = True

    def cast_weight(dst, dst_slices, src_aps, stage, eng_cycle=("pool", "dve")):
        for i, (dsl, sap) in enumerate(zip(dst_slices, src_aps)):
            sg = stage[i % len(stage)]
            shp = sap.shape
            view = sg[0:shp[0], 0:shp[1]]
            P.dma(view, sap, writes=[sg])
            P.v("tensor_copy", dsl, view, reads=[sg], writes=[dst], eng=eng_cycle[i % len(eng_cycle)])

    def rms_rstd(src, nk, n, inv_dim, rstd, sq, alloc=None):
        P.act(sq[:, 0:nk, 0:n], src[:, 0:nk, 0:n], AF.Square, reads=[src], writes=[sq])
        ps = nps() if alloc is None else alloc.nps()
        for k in range(nk):
            P.mm(ps[:, 0:n], ONES, sq[:, k, 0:n], start=(k == 0), stop=(k == nk - 1), reads=[cf, sq], writes=[ps])
        P.act(rstd[:, 0:n], ps[:, 0:n], AF.Sqrt, scale=inv_dim, bias=epsb[:, 0:1], reads=[ps, epsb], writes=[rstd])
        P.v("reciprocal", rstd[:, 0:n], rstd[:, 0:n], reads=[rstd], writes=[rstd])

    def chunks(seg):
        c0, ln = SEGS[seg]
        return [(c0 + o, min(512, ln - o)) for o in range(0, ln, 512)]

    for l in range(nl):
        last = (l == NL - 1)
        def mod(which):
            return mods[:, l, which * 8:(which + 1) * 8, :]

        def vcol(off, n=8):
            return vec[:, l, off:off + n].unsqueeze(2).to_broadcast([128, n, 2])
        P.v("scalar_tensor_tensor", drv[:, 0], mod(1), 1.0, vcol(V_NMP), ALU.add, ALU.mult, reads=[mods, vec], writes=[drv])
        P.v("tensor_tensor", drv[:, 1], mod(2), vcol(V_NMO), ALU.mult, reads=[mods, vec], writes=[drv])
        P.v("scalar_tensor_tensor", drv[:, 2], mod(4), 1.0, vcol(V_NFP), ALU.add, ALU.mult, reads=[mods, vec], writes=[drv])
        P.v("tensor_tensor", drv[:, 3], mod(5), vcol(V_NFO), ALU.mult, reads=[mods, vec], writes=[drv])

        ph("A")
        AR.reset()
        W1b = AR.alloc([128, 8, NW1], BF16, "W1b")
        WUQb = AR.alloc([128, 3, 1024], BF16, "WUQb")
        WKb = AR.alloc([128, 2, 768], BF16, "WKb")
        WVb = AR.alloc([128, 2, 512], BF16, "WVb")
        stage = [AR.alloc([128, NW1], F32, f"stg{i}") for i in range(2)]
        cast_weight(W1b, [W1b[:, k, :] for k in range(8)],
                    [w1_d[l, k * 128:(k + 1) * 128, :] for k in range(8)], stage)
        cast_weight(WUQb, [WUQb[:, k, :] for k in range(3)],
                    [wuq_d[l, k * 128:(k + 1) * 128, :] for k in range(3)], stage)
        cast_weight(WKb, [WKb[:, k, :] for k in range(2)],
                    [wukvk_d[l, k * 128:(k + 1) * 128, :] for k in range(2)], stage)
        cast_weight(WVb, [WVb[:, k, :] for k in range(2)],
                    [wukvv_d[l, k * 128:(k + 1) * 128, :] for k in range(2)], stage)
        xs = AR.alloc([128, 8, 512], F32, "xs")
        sq = AR.alloc([128, 8, 512], F32, "sq")
        hb = AR.alloc([128, 8, 512], BF16, "hb")
        rstd = AR.alloc([128, 512], F32, "rstd")
        cqf = AR.alloc([128, 5, 512], F32, "cqf")
        cqs = Tl(sq.ap[:, 0:5], sq.key)
        cqn = AR.alloc([128, 5, 512], BF16, "cqn")
        rs2 = AR.alloc([128, 2, 512], F32, "rs2")
        ropeq = AR.alloc([96, 2, 512], F32, "ropeq")
        ropek = AR.alloc([32, 2, 512], F32, "ropek")
        kra = AR.alloc([32, 2, 512], F32, "kra")
        krot = AR.alloc([32, 512], BF16, "krot")
        qst = [AR.alloc([96, 512], BF16, f"qst{i}") for i in range(2)]
        qt1 = AR.alloc([96, 2, 512], F32, "qt1")
        kst = [AR.alloc([96, 512], BF16, f"kst{i}") for i in range(2)]
        vst = [AR.alloc([128, 8, 65], BF16, f"vst{i}") for i in range(2)]
        fst = [AR.alloc([128, 512], BF16, f"fst{i}") for i in range(3)]
        lst = AR.alloc([64, 512], F32, "lst")
        tmb = [AR.alloc([128, 1024], BF16, f"tmb{i}") for i in range(2)]
        tmf = [AR.alloc([128, 16], F32, f"tmf{i}") for i in range(2)]
        for t_ in vst:
            P.v("memset", t_[:], 1.0, writes=[t_])
        cnt = 0
        for seg in (0, 1):
            for (c0, n) in chunks(seg):
                lat = (seg == 1)
                mi = 1 - seg
                P.dma(xs[:, :, 0:n], XT[:, c0:c0 + n].rearrange("(k p) t -> p k t", p=128), writes=[xs])
                rms_rstd(xs, 8, n, 1.0 / D, rstd, sq)
                P.v("tensor_tensor", sq[:, :, 0:n], xs[:, :, 0:n], rstd[:, 0:n].unsqueeze(1).to_broadcast([128, 8, n]),
                    ALU.mult, reads=[xs, rstd], writes=[sq])
                for k in range(8):
                    P.act(hb[:, k, 0:n], sq[:, k, 0:n], AF.Identity, scale=drv[:, 0, k, mi:mi + 1],
                          bias=mods[:, l, k, mi:mi + 1], reads=[sq, drv, mods], writes=[hb])
                if c0 == TC:
                    dump("d_hb", hb, [128, 8, 512], BF16)
                    dump("d_rstd", rstd, [128, 512])
                    dump("d_xs", xs, [128, 8, 512])
                if lat:
                    t0 = c0 - TC
                    P.dma(ropeq[64:96, :, 0:n], rope_d[:, :, t0:t0 + n].rearrange("s p t -> p s t"), writes=[ropeq])
                    P.dma(ropek[:, :, 0:n], rope_d[:, :, t0:t0 + n].rearrange("s p t -> p s t"), writes=[ropek])

                def fm_group(col, rows):
                    ps = nps()
                    for k in range(8):
                        P.mm(ps[0:rows, 0:n], W1b[:, k, col:col + rows], hb[:, k, 0:n], start=(k == 0), stop=(k == 7),
                             reads=[W1b, hb], writes=[ps])
                    return ps
                for g in range(5):
                    ps = fm_group(g * 128, 128)
                    P.act(cqf[:, g, 0:n], ps[:, 0:n], AF.Copy, reads=[ps], writes=[cqf])
                rms_rstd(Tl(cqf[:, 0:3], cqf.key), 3, n, 1.0 / 384, Tl(rs2[:, 0], rs2.key), Tl(cqs[:, 0:3], cqs.key))
                rms_rstd(Tl(cqf[:, 3:5], cqf.key), 2, n, 1.0 / 256, Tl(rs2[:, 1], rs2.key), Tl(cqs[:, 3:5], cqs.key))
                for g in range(5):
                    r = 0 if g < 3 else 1
                    P.v("tensor_tensor", cqs[:, g, 0:n], cqf[:, g, 0:n], rs2[:, r, 0:n], ALU.mult, reads=[cqf, rs2], writes=[cqs])
                    P.act(cqn[:, g, 0:n], cqs[:, g, 0:n], AF.Identity, scale=vec[:, l, V_QN + g:V_QN + g + 1],
                          reads=[cqs, vec], writes=[cqn])
                psa = fm_group(640, 32)
                if lat:
                    psb = fm_group(672, 32)
                    P.v("tensor_tensor", kra[:, 0, 0:n], psa[0:32, 0:n], ropek[:, 0, 0:n], ALU.mult, reads=[psa, ropek], writes=[kra])
                    P.v("tensor_tensor", kra[:, 1, 0:n], psb[0:32, 0:n], ropek[:, 1, 0:n], ALU.mult, reads=[psb, ropek], writes=[kra])
                    P.v("tensor_tensor", krot[:, 0:n], kra[:, 0, 0:n], kra[:, 1, 0:n], ALU.add, reads=[kra], writes=[krot])
                else:
                    P.act(krot[:, 0:n], psa[0:32, 0:n], AF.Copy, reads=[psa], writes=[krot])
                for h in range(8):
                    pa = nps()
                    for k in range(3):
                        P.mm(pa[0:96, 0:n], WUQb[:, k, h * 128:h * 128 + 96], cqn[:, k, 0:n], start=(k == 0), stop=(k == 2),
                             reads=[WUQb, cqn], writes=[pa])
                    q_ = qst[cnt % 2]
                    if lat:
                        pb = nps()
                        for k in range(3):
                            P.mm(pb[64:96, 0:n], WUQb[:, k, h * 128 + 96:h * 128 + 128], cqn[:, k, 0:n], start=(k == 0),
                                 stop=(k == 2), reads=[WUQb, cqn], writes=[pb])
                        P.act(q_[0:64, 0:n], pa[0:64, 0:n], AF.Copy, reads=[pa], writes=[q_])
                        P.v("tensor_tensor", qt1[64:96, 0, 0:n], pa[64:96, 0:n], ropeq[64:96, 0, 0:n], ALU.mult,
                            reads=[pa, ropeq], writes=[qt1])
                        P.v("tensor_tensor", qt1[64:96, 1, 0:n], pb[64:96, 0:n], ropeq[64:96, 1, 0:n], ALU.mult,
                            reads=[pb, ropeq], writes=[qt1])
                        P.v("tensor_tensor", q_[64:96, 0:n], qt1[64:96, 0, 0:n], qt1[64:96, 1, 0:n], ALU.add,
                            reads=[qt1], writes=[q_])
                    else:
                        P.act(q_[0:96, 0:n], pa[0:96, 0:n], AF.Copy, reads=[pa], writes=[q_])
                    P.dma(QT[h, :, c0:c0 + n], q_[0:96, 0:n], reads=[q_], q="pool")
                    pk = nps()
                    for k in range(2):
                        P.mm(pk[0:96, 0:n], WKb[:, k, h * 96:(h + 1) * 96], cqn[:, 3 + k, 0:n], start=(k == 0), stop=False,
                             reads=[WKb, cqn], writes=[pk])
                    P.mm(pk[0:96, 0:n], eropeb[:], krot[:, 0:n], start=False, stop=True, reads=[eropeb, krot], writes=[pk])
                    k_ = kst[cnt % 2]
                    P.act(k_[0:96, 0:n], pk[0:96, 0:n], AF.Copy, reads=[pk], writes=[k_])
                    P.dma(KT[h, :, c0:c0 + n], k_[0:96, 0:n], reads=[k_], q="pool")
                    cnt += 1
                for j in range(n // 128):
                    tsl = slice(j * 128, (j + 1) * 128)
                    r0 = c0 + j * 128
                    pv = nps()
                    for k in range(2):
                        P.mm(pv[:, 0:512], cqn[:, 3 + k, tsl], WVb[:, k, :], start=(k == 0), stop=(k == 1),
                             reads=[cqn, WVb], writes=[pv])
                    v_ = vst[cnt % 2]
                    P.act(v_[:, :, 0:64], pv[:, 0:512].rearrange("p (h d) -> p h d", d=64), AF.Copy, reads=[pv], writes=[v_])
                    P.dma(VT[r0:r0 + 128, :], v_[:].rearrange("p h d -> p (h d)"), reads=[v_], q="pool")
                    tb = tmb[cnt % 2]
                    tf = tmf[cnt % 2]
                    for gg, (col, wd) in enumerate(((1792, 512), (2304, 512), (2816, 16))):
                        pt = nps()
                        for k in range(8):
                            P.mm(pt[:, 0:wd], hb[:, k, tsl], W1b[:, k, col:col + wd], start=(k == 0), stop=(k == 7),
                                 reads=[hb, W1b], writes=[pt])
                        if gg < 2:
                            if gg == 0:
                                P.act(tb[:, 0:512], pt[:, 0:512], AF.Copy, reads=[pt], writes=[tb])
                            else:
                                P.v("tensor_copy", tb[:, 512:1024], pt[:, 0:512], reads=[pt], writes=[tb])
                        else:
                            P.v("tensor_copy", tf[:], pt[:, 0:16], reads=[pt], writes=[tf])
                    P.dma(TMB[r0:r0 + 128, :], tb[:], reads=[tb], q="pool")
                    P.dma(TMF[r0:r0 + 128, :], tf[:], reads=[tf], q="pool")
                    cnt += 1
                for g in range(8):
                    ps = fm_group(704 + g * 128, 128)
                    f_ = fst[g % 3]
                    if g % 2 == 0:
                        P.act(f_[:, 0:n], ps[:, 0:n], AF.Copy, reads=[ps], writes=[f_])
                    else:
                        P.v("tensor_copy", f_[:, 0:n], ps[:, 0:n], reads=[ps], writes=[f_])
                    if g < 6:
                        P.dma(GQKV[g * 128:(g + 1) * 128, c0:c0 + n], f_[:, 0:n], reads=[f_], q="pool")
                    else:
                        P.dma(LQKT[(g - 6) * 128:(g - 5) * 128, c0:c0 + n], f_[:, 0:n], reads=[f_], q="pool")
                ps = fm_group(1728, 64)
                P.v("tensor_copy", lst[:, 0:n], ps[0:64, 0:n], reads=[ps], writes=[lst])
                P.dma(LRT[:, c0:c0 + n], lst[:, 0:n], reads=[lst], q="pool")
        P.barrier()
        if stop_after == "A":
            break
        ph("MLA")
        AR.reset()
        Ksb = AR.alloc([96, 8, TT], BF16, "Ksb")
        Vsb = AR.alloc([128, NT, 520], BF16, "Vsb")
        for h in range(8):
            P.dma(Ksb[:, h, :], KT[h], writes=[Ksb])
        for j0 in range(0, NT, 6):
            j1 = min(NT, j0 + 6)
            P.dma(Vsb[:, j0:j1, :], VT[j0 * 128:j1 * 128, :].rearrange("(j p) c -> p j c", p=128), writes=[Vsb])
        qsb = [AR.alloc([96, 8, 512], BF16, f"qsb{i}") for i in range(2)]
        ptl = [AR.alloc([128, 2, 512], BF16, f"pt{i}") for i in range(3)]
        rc = AR.alloc([128, 512], F32, "rc")
        osb = [AR.alloc([64, 512], F32, f"osb{i}") for i in range(2)]
        omb = [AR.alloc([64, 512], BF16, f"om{i}") for i in range(2)]
        SCALE = float(96 ** -0.5)
        actr = [0, 0, 0]

        def nps_small():
            b = 2 * (actr[2] % 3)
            actr[2] += 1
            return Tl(pst[:, b, :], PSK[b])

        def attend(q0, nq, ktiles):
            qs = qsb[actr[0] % 2]
            actr[0] += 1
            P.dma(qs[:, :, 0:nq], QT[:, :, q0:q0 + nq].rearrange("h p t -> p h t"), writes=[qs])
            pairs = [ktiles[i:i + 2] for i in range(0, len(ktiles), 2)]
            units = [(h, pi, pr) for h in range(8) for pi, pr in enumerate(pairs)]

            def qk(u):
                h, pi, pr = units[u]
                b = 2 * (actr[2] % 3)
                p_ = ptl[actr[2] % 3]
                actr[2] += 1
                for jj, j in enumerate(pr):
                    P.mm(pst[:, b + jj, 0:nq], Ksb[:, h, j * 128:(j + 1) * 128], qs[:, h, 0:nq],
                         reads=[Ksb, qs], writes=[PSK[b + jj]])
                return b, p_

            nxt = qk(0)
            po = None
            for u, (h, pi, pr) in enumerate(units):
                b, p_ = nxt
                if pi == 0:
                    pob = 6 + (actr[1] % 2)
                    actr[1] += 1
                    po = Tl(pst[:, pob, :], PSK[pob])
                if u + 1 < len(units):
                    nxt = qk(u + 1)
                P.act(p_[:, 0:len(pr), 0:nq], pst[:, b:b + len(pr), 0:nq], AF.Exp, scale=SCALE,
                      reads=[PSK[b], PSK[b + 1]], writes=[p_])
                for jj, j in enumerate(pr):
                    P.mm(po[0:65, 0:nq], Vsb[:, j, h * 65:(h + 1) * 65], p_[:, jj, 0:nq],
                         start=(pi == 0 and jj == 0), stop=(pi == len(pairs) - 1 and jj == len(pr) - 1),
                         reads=[Vsb, p_], writes=[po])
                if pi == len(pairs) - 1:
                    P.v("reciprocal", rc[64:65, 0:nq], po[64:65, 0:nq], reads=[po], writes=[rc])
                    pb = nps_small()
                    P.mm(pb[0:64, 0:nq], cf[64:65, C_ONES:C_ONES + 64], rc[64:65, 0:nq], reads=[cf, rc], writes=[pb])
                    o_ = osb[h % 2]
                    m_ = omb[h % 2]
                    P.v("tensor_copy", o_[:, 0:nq], po[0:64, 0:nq], reads=[po], writes=[o_])
                    P.v("tensor_tensor", m_[:, 0:nq], o_[:, 0:nq], pb[0:64, 0:nq], ALU.mult, reads=[o_, pb], writes=[m_])
                    P.dma(MT[h * 64:(h + 1) * 64, q0:q0 + nq], m_[:, 0:nq], reads=[m_], q="pool")

        if not last:
            attend(0, TC, [0, 1])
        for qc in range(TX // 512):
            attend(TC + qc * 512, 512, list(range(NT)))
        P.barrier()
        if stop_after == "MLA":
            break
        ph("GPREP")
        AR.reset()
        raw = [AR.alloc([128, 6, 514], BF16, f"raw{i}") for i in range(2)]
        cvt = AR.alloc([128, 6, 512], F32, "cvt")
        qkvc = AR.alloc([128, 6, 512], BF16, "qkvc")
        sqk = AR.alloc([128, 512], F32, "sqk")
        ssq = AR.alloc([128, 8], F32, "ssq")
        gnt_ = [AR.alloc([128, 768], BF16, f"gnt{i}") for i in range(2)]
        gnT_ = [AR.alloc([128, 512], BF16, f"gnT{i}") for i in range(2)]
        tf_ = [AR.alloc([128, 16], F32, f"tf{i}") for i in range(2)]
        gg_ = [AR.alloc([128, 24], F32, f"gg{i}") for i in range(2)]
        t8 = AR.alloc([128, 8], F32, "t8")
        nega = AR.alloc([128, 8], F32, "nega")
        P.act(nega[:], bcv[:, l, B_ALOG:B_ALOG + 8], AF.Exp, reads=[bcv], writes=[nega])
        P.v("tensor_scalar", nega[:], nega[:], -1.0, None, ALU.mult, reads=[nega], writes=[nega])
        ci = 0
        ti = 0
        for seg in (0, 1):
            s0, ln = SEGS[seg]
            for (c0, n) in chunks(seg):
                rw = raw[ci % 2]
                ci += 1
                lo = max(c0 - 1, s0)
                hi = min(c0 + n + 1, s0 + ln)
                P.dma(rw[:, :, lo - c0 + 1:hi - c0 + 1], GQKV[:, lo:hi].rearrange("(g p) t -> p g t", p=128), writes=[rw])
                if lo == c0:
                    P.v("memset", rw[:, :, 0:1], 0.0, writes=[rw], eng="pool")
                if hi == c0 + n:
                    P.v("memset", rw[:, :, n + 1:n + 2], 0.0, writes=[rw], eng="pool")
                for g in range(6):
                    wv = lambda t: vec[:, l, V_GCW + 6 * t + g:V_GCW + 6 * t + g + 1]
                    P.v("tensor_scalar", cvt[:, g, 0:n], rw[:, g, 0:n], wv(0), None, ALU.mult, reads=[rw, vec], writes=[cvt])
                    P.v("scalar_tensor_tensor", cvt[:, g, 0:n], rw[:, g, 1:n + 1], wv(1), cvt[:, g, 0:n], ALU.mult, ALU.add,
                        reads=[rw, vec, cvt], writes=[cvt])
                    P.v("scalar_tensor_tensor", cvt[:, g, 0:n], rw[:, g, 2:n + 2], wv(2), cvt[:, g, 0:n], ALU.mult, ALU.add,
                        reads=[rw, vec, cvt], writes=[cvt])
                    P.act(qkvc[:, g, 0:n], cvt[:, g, 0:n], AF.Silu, reads=[cvt], writes=[qkvc])
                for j in range(n // 128):
                    r0 = c0 + j * 128
                    gt = gnt_[ti % 2]
                    gT = gnT_[ti % 2]
                    tf = tf_[ti % 2]
                    gg = gg_[ti % 2]
                    ti += 1
                    pt = nps()
                    ptb = pt.ap.bitcast(BF16)
                    for g in range(6):
                        P.tr(ptb[:, g * 128:(g + 1) * 128], qkvc[:, g, j * 128:(j + 1) * 128], identb[:],
                             reads=[qkvc, identb], writes=[pt])
                    P.act(sqk[:], ptb[:, 0:512], AF.Square, reads=[pt], writes=[sqk])
                    P.v("tensor_reduce", ssq[:], sqk[:].rearrange("p (g d) -> p g d", d=64), AX.X, ALU.add, reads=[sqk], writes=[ssq])
                    P.act(ssq[:], ssq[:], AF.Sqrt, bias=epsb[:, 0:1], reads=[ssq, epsb], writes=[ssq])
                    P.v("reciprocal", ssq[:], ssq[:], reads=[ssq], writes=[ssq])
                    P.v("tensor_scalar", ssq[:, 0:4], ssq[:, 0:4], 0.125, None, ALU.mult, reads=[ssq], writes=[ssq])
                    P.v("tensor_tensor", gt[:, 0:512].rearrange("p (g d) -> p g d", d=64),
                        ptb[:, 0:512].rearrange("p (g d) -> p g d", d=64),
                        ssq[:].unsqueeze(2).to_broadcast([128, 8, 64]), ALU.mult, reads=[pt, ssq], writes=[gt])
                    P.act(gt[:, 512:768], ptb[:, 512:768], AF.Copy, reads=[pt], writes=[gt])
                    P.dma(GN[r0:r0 + 128, :], gt[:], reads=[gt], q="pool")
                    pt2 = nps()
                    pt2b = pt2.ap.bitcast(BF16)
                    for c in range(4):
                        P.tr(pt2b[:, c * 128:(c + 1) * 128], gt[:, c * 128:(c + 1) * 128], identb[:],
                             reads=[gt, identb], writes=[pt2])
                    P.v("tensor_copy", gT[:], pt2b[:, 0:512], reads=[pt2], writes=[gT])
                    P.dma(GNT[r0 // 128], gT[:], reads=[gT], q="pool")
                    P.dma(tf[:], TMF[r0:r0 + 128, :], writes=[tf])
                    P.v("tensor_tensor", t8[:], tf[:, 0:8], bcv[:, l, B_DTB:B_DTB + 8], ALU.add, reads=[tf, bcv], writes=[t8])
                    P.act(t8[:], t8[:], AF.Exp, reads=[t8], writes=[t8])
                    P.act(t8[:], t8[:], AF.Ln, bias=oneb[:, 0:1], reads=[t8, oneb], writes=[t8])
                    P.v("tensor_tensor", gg[:, 0:8], t8[:], nega[:], ALU.mult, reads=[t8, nega], writes=[gg])
                    P.act(gg[:, 8:16], tf[:, 8:16], AF.Sigmoid, reads=[tf], writes=[gg])
                    P.act(gg[:, 16:24], gg[:, 8:16], AF.Ln, reads=[gg], writes=[gg])
                    P.dma(GG[r0:r0 + 128, :], gg[:], reads=[gg], q="pool")
        P.barrier()
        if stop_after == "GPREP":
            break
        ph("GDN")
        AR.reset()
        ID4 = IDENT.unsqueeze(1).to_broadcast([128, 4, 128])
        BD4 = cf[:, C_BD:C_BD + 128].unsqueeze(1).to_broadcast([128, 4, 128])

        def b4(ap, w):
            return ap.unsqueeze(2).to_broadcast([128, 4, w])

        def nat4(ap):
            return ap.rearrange("p (c q) x -> p q c x", q=2)

        def v4(ps):
            return ps[:, 0:512].rearrange("p (h i) -> p h i", h=4)

        def scan_order(d):
            o = [0, 1] + list(range(2, NT)) if d == 0 else [1, 0] + list(range(NT - 1, 1, -1))
            return o if GDN_TILES is None else o[:GDN_TILES]

        def gdn_chain(d):
            sfx = f"_{d}"
            PA_ = PSA([4 * d + i for i in range(4)])
            gn_l = [AR.alloc([128, 768], BF16, f"gnl{i}" + sfx) for i in range(2)]
            gT_l = [AR.alloc([128, 4, 128], BF16, f"gTl{i}" + sfx) for i in range(2)]
            gg_l = [AR.alloc([128, 24], F32, f"ggl{i}" + sfx) for i in range(2)]
            gsm = AR.alloc([128, 40], F32, "gsm" + sfx)
            Rt = AR.alloc([128, 4, 128], F32, "Rt" + sfx)
            RBt = AR.alloc([128, 4, 128], F32, "RBt" + sfx)
            DT = AR.alloc([128, 4, 128], F32, "DT" + sfx)
            DBT = AR.alloc([128, 4, 128], F32, "DBT" + sfx)
            ATb = AR.alloc([128, 4, 128], BF16, "ATb" + sfx)
            Nf = AR.alloc([128, 4, 128], F32, "Nf" + sfx)
            Xf = AR.alloc([128, 4, 128], F32, "Xf" + sfx)
            Xb = AR.alloc([128, 4, 128], BF16, "Xb" + sfx)
            Pb = [AR.alloc([128, 4, 128], F32, f"Pb{i}" + sfx) for i in range(2)]
            Qb = [AR.alloc([128, 4, 128], F32, f"Qb{i}" + sfx) for i in range(2)]
            Loff = AR.alloc([128, 4, 128], F32, "Loff" + sfx)
            Ysb = AR.alloc([128, 4, 128], F32, "Ysb" + sfx)
            Xts = AR.alloc([128, 4, 128], F32, "Xts" + sfx)
            vb = AR.alloc([128, 4, 64], BF16, "vb" + sfx)
            kbg = AR.alloc([128, 4, 64], BF16, "kbg" + sfx)
            kend = AR.alloc([128, 4, 64], BF16, "kend" + sfx)
            wT = AR.alloc([128, 2, 128], BF16, "wT" + sfx)
            usb = AR.alloc([128, 256], F32, "usb" + sfx)
            vnew = AR.alloc([128, 256], BF16, "vnew" + sfx)
            o1 = AR.alloc([128, 256], F32, "o1" + sfx)
            ot = [AR.alloc([128, 256], F32, f"ot{i}" + sfx) for i in range(2)]
            Sf = AR.alloc([128, 2, 64], F32, "Sf" + sfx)
            Sb = AR.alloc([128, 2, 64], BF16, "Sb" + sfx)
            gc, ngc, eg, egl, ekd, bg = (gsm[:, 4 * i:4 * i + 4] for i in range(6))
            egls = gsm[:, 24:26]
            TRI = cf[:, C_TRI0 + 128 * d:C_TRI0 + 128 * d + 128]
            NMI = cf[:, C_NM + (2 * d) * 512:C_NM + (2 * d) * 512 + 512]
            NMS = cf[:, C_NM + (2 * d + 1) * 512:C_NM + (2 * d + 1) * 512 + 512]
            P.v("memset", Sf[:], 0.0, writes=[Sf])
            P.v("memset", Sb[:], 0.0, writes=[Sb])
            yield
            li = 0
            for t in scan_order(d):
                r0 = t * 128
                gn = gn_l[li % 2]
                gT = gT_l[li % 2]
                gg = gg_l[li % 2]
                li += 1
                P.dma(gn[:], GN[r0:r0 + 128, :], writes=[gn])
                P.dma(gT[:], GNT[t].rearrange("p (c t) -> p c t", c=4), writes=[gT])
                P.dma(gg[:], GG[r0:r0 + 128, :], writes=[gg])
                gd = gg[:, 4 * d:4 * d + 4]
                beta = gg[:, 8 + 4 * d:12 + 4 * d]
                lnb = gg[:, 16 + 4 * d:20 + 4 * d]
                kv = gn[:, 256:512].rearrange("p (h d) -> p h d", d=64)
                vv = gn[:, 512:768].rearrange("p (h d) -> p h d", d=64)
                pg = PA_.nps()
                P.mm(pg[:, 0:4], TRI, gd, reads=[cf, gg], writes=[pg])
                P.mm(pg[:, 4:8], ONES, gd, reads=[cf, gg], writes=[pg])
                P.v("tensor_tensor", Rt[:], b4(gd, 128), TRI.unsqueeze(1).to_broadcast([128, 4, 128]), ALU.mult,
                    reads=[gg, cf], writes=[Rt])
                P.v("tensor_tensor", RBt[:], b4(lnb, 128), ID4, ALU.mult, reads=[gg, cf], writes=[RBt])
                P.v("tensor_tensor", RBt[:], RBt[:], Rt[:], ALU.add, reads=[RBt, Rt], writes=[RBt])
                yield
                P.v("tensor_copy", gc, pg[:, 0:4], reads=[pg], writes=[gsm])
                P.v("tensor_scalar", ngc, pg[:, 0:4], -1.0, None, ALU.mult, reads=[pg], writes=[gsm])
                P.act(eg, pg[:, 0:4], AF.Exp, reads=[pg], writes=[gsm])
                P.act(egl, pg[:, 4:8], AF.Exp, reads=[pg], writes=[gsm])
                P.v("tensor_tensor", ekd, pg[:, 4:8], gc, ALU.subtract, reads=[pg, gsm], writes=[gsm])
                ps1 = PA_.nps()
                P.mm(ps1[:, 0:512], ONES, Rt[:].rearrange("p h i -> p (h i)"), start=True, stop=False, reads=[cf, Rt], writes=[ps1])
                P.mm(ps1[:, 0:512], IDENT, NMI, start=False, stop=True, reads=[cf], writes=[ps1])
                ps2 = PA_.nps()
                P.mm(ps2[:, 0:512], ONES, RBt[:].rearrange("p h i -> p (h i)"), start=True, stop=False, reads=[cf, RBt], writes=[ps2])
                P.mm(ps2[:, 0:512], IDENT, NMS, start=False, stop=True, reads=[cf], writes=[ps2])
                yield
                P.act(ekd, ekd, AF.Exp, reads=[gsm], writes=[gsm])
                P.v("tensor_tensor", bg, beta, eg, ALU.mult, reads=[gg, gsm], writes=[gsm])
                P.v("tensor_copy", egls[0:64, :], egl[0:64, 0:4:2], reads=[gsm], writes=[gsm])
                P.v("tensor_copy", egls[64:128, :], egl[64:128, 1:4:2], reads=[gsm], writes=[gsm])
                for h in range(4):
                    P.act(DT[:, h, :], ps1[:, h * 128:(h + 1) * 128], AF.Exp, bias=ngc[:, h:h + 1], reads=[ps1, gsm], writes=[DT])
                    P.act(DBT[:, h, :], ps2[:, h * 128:(h + 1) * 128], AF.Exp, bias=ngc[:, h:h + 1], reads=[ps2, gsm], writes=[DBT])
                bkk = PA_.nps2()
                bqk = PA_.nps2()
                for h in (0, 2, 1, 3):
                    par, c = h % 2, h // 2
                    hp = par * 64
                    kT = gT[hp:hp + 64, 2 + c, :]
                    qT = gT[hp:hp + 64, c, :]
                    P.mm(pst[:, bkk + par, c * 128:(c + 1) * 128], kT, kT, reads=[gT], writes=[PSK[bkk + par]])
                    P.mm(pst[:, bqk + par, c * 128:(c + 1) * 128], kT, qT, reads=[gT], writes=[PSK[bqk + par]])
                yield
                P.v("tensor_tensor", nat4(Nf[:]), pv2(bkk, 128), nat4(DBT[:]), ALU.mult,
                    reads=[PSK[bkk], PSK[bkk + 1], DBT], writes=[Nf])
                P.v("tensor_tensor", nat4(ATb[:]), pv2(bqk, 128), nat4(DT[:]), ALU.mult,
                    reads=[PSK[bqk], PSK[bqk + 1], DT], writes=[ATb])
                P.v("tensor_tensor", Pb[0][:], Nf[:], BD4, ALU.mult, reads=[Nf, cf], writes=[Pb[0]])
                P.v("tensor_tensor", Xf[:], ID4, Pb[0][:], ALU.subtract, reads=[cf, Pb[0]], writes=[Xf])
                pl = PA_.nps()
                for h in range(4):
                    P.tr(pl[:, h * 128:(h + 1) * 128], Nf[:, h, :], IDENT, reads=[Nf, cf], writes=[pl])
                yield
                P.v("tensor_tensor", Qb[0][:], v4(pl), BD4, ALU.mult, reads=[pl, cf], writes=[Qb[0]])
                P.v("tensor_tensor", Loff[:], v4(pl), Qb[0][:], ALU.subtract, reads=[pl, Qb[0]], writes=[Loff])
                for it in range(5):
                    Pc, Qc = Pb[it % 2], Qb[it % 2]
                    Pn, Qn = Pb[(it + 1) % 2], Qb[(it + 1) % 2]
                    pQ = PA_.nps()
                    for h in range(4):
                        P.mm(pQ[:, h * 128:(h + 1) * 128], Pc[:, h, :], Qc[:, h, :], reads=[Pc, Qc], writes=[pQ])
                    if it < 4:
                        pP = PA_.nps()
                        for h in range(4):
                            P.mm(pP[:, h * 128:(h + 1) * 128], Qc[:, h, :], Pc[:, h, :], reads=[Pc, Qc], writes=[pP])
                    yield
                    P.v("tensor_copy", Qn[:], v4(pQ), reads=[pQ], writes=[Qn])
                    if it < 4:
                        P.act(Pn[:], v4(pP), AF.Copy, reads=[pP], writes=[Pn])
                    pX = PA_.nps()
                    for h in range(4):
                        P.mm(pX[:, h * 128:(h + 1) * 128], Qn[:, h, :], Xf[:, h, :], reads=[Qn, Xf], writes=[pX])
                    yield
                    P.v("tensor_tensor", Xf[:], Xf[:], v4(pX), ALU.add, reads=[Xf, pX], writes=[Xf])
                pY = PA_.nps()
                pT = PA_.nps()
                for h in range(4):
                    P.mm(pY[:, h * 128:(h + 1) * 128], Loff[:, h, :], Xf[:, h, :], reads=[Loff, Xf], writes=[pY])
                    P.tr(pT[:, h * 128:(h + 1) * 128], Xf[:, h, :], IDENT, reads=[Xf, cf], writes=[pT])
                P.v("tensor_tensor", vb[:], vv, b4(beta, 64), ALU.mult, reads=[gn, gg], writes=[vb], eng="pool")
                P.v("tensor_tensor", kbg[:], kv, b4(bg, 64), ALU.mult, reads=[gn, gsm], writes=[kbg], eng="pool")
                P.v("tensor_tensor", kend[:], kv, b4(ekd, 64), ALU.mult, reads=[gn, gsm], writes=[kend], eng="pool")
                yield
                P.act(Ysb[:], v4(pY), AF.Copy, reads=[pY], writes=[Ysb])
                P.v("tensor_copy", Xts[:], v4(pT), reads=[pT], writes=[Xts])
                pZ = PA_.nps()
                for h in range(4):
                    P.mm(pZ[:, h * 128:(h + 1) * 128], Xts[:, h, :], Ysb[:, h, :], reads=[Xts, Ysb], writes=[pZ])
                yield
                P.v("tensor_tensor", Xb[:], Xf[:], v4(pZ), ALU.subtract, reads=[Xf, pZ], writes=[Xb])
                pu = PA_.nps()
                pw = PA_.nps()
                for h in range(4):
                    hp = (h % 2) * 64
                    P.mm(pu[:, h * 64:(h + 1) * 64], Xb[:, h, :], vb[:, h, :], reads=[Xb, vb], writes=[pu])
                    P.mm(pw[hp:hp + 64, (h // 2) * 128:(h // 2 + 1) * 128], kbg[:, h, :], Xb[:, h, :], reads=[Xb, kbg], writes=[pw])
                yield
                P.act(usb[:], pu[:, 0:256], AF.Copy, reads=[pu], writes=[usb])
                P.act(wT[:], pw[:, 0:256].rearrange("p (c i) -> p c i", c=2), AF.Copy, reads=[pw], writes=[wT])
                bws = PA_.nps2()
                bq = PA_.nps2()
                for h in (0, 2, 1, 3):
                    par, c = h % 2, h // 2
                    hp = par * 64
                    P.mm(pst[:, bws + par, c * 64:(c + 1) * 64], wT[hp:hp + 64, c, :], Sb[hp:hp + 64, c, :], reads=[wT, Sb],
                         writes=[PSK[bws + par]])
                    P.mm(pst[:, bq + par, c * 64:(c + 1) * 64], gT[hp:hp + 64, c, :], Sb[hp:hp + 64, c, :], reads=[gT, Sb],
                         writes=[PSK[bq + par]])
                yield
                P.v("tensor_tensor", nat4(vnew[:].rearrange("p (h d) -> p h d", d=64)), nat4(usb[:].rearrange("p (h d) -> p h d", d=64)),
                    pv2(bws, 64), ALU.subtract, reads=[usb, PSK[bws], PSK[bws + 1]], writes=[vnew])
                P.v("tensor_tensor", nat4(o1[:].rearrange("p (h d) -> p h d", d=64)), pv2(bq, 64),
                    nat4(b4(eg, 64)), ALU.mult, reads=[PSK[bq], PSK[bq + 1], gsm], writes=[o1])
                po = PA_.nps()
                pS = PA_.nps()
                for h in range(4):
                    hp = (h % 2) * 64
                    P.mm(po[:, h * 64:(h + 1) * 64], ATb[:, h, :], vnew[:, h * 64:(h + 1) * 64], reads=[ATb, vnew], writes=[po])
                    P.mm(pS[hp:hp + 64, (h // 2) * 64:(h // 2 + 1) * 64], kend[:, h, :], vnew[:, h * 64:(h + 1) * 64],
                         reads=[kend, vnew], writes=[pS])
                o_ = ot[li % 2]
                P.v("tensor_tensor", Sf[:], Sf[:], egls.unsqueeze(2).to_broadcast([128, 2, 64]), ALU.mult, reads=[Sf, gsm], writes=[Sf])
                yield
                P.v("tensor_tensor", o_[:], o1[:], po[:, 0:256], ALU.add, reads=[o1, po], writes=[o_])
                P.dma(GO[d, r0:r0 + 128, :], o_[:], reads=[o_], q="pool")
                P.v("tensor_tensor", Sf[:], Sf[:], pS[:, 0:128].rearrange("p (c v) -> p c v", c=2), ALU.add, reads=[Sf, pS], writes=[Sf])
                P.v("tensor_copy", Sb[:], Sf[:], reads=[Sf], writes=[Sb], eng="pool")
                yield

        wgk_sb = AR.alloc([64, 128], F32, "wgk")
        P.dma(wgk_sb[:], wgk_d[l], writes=[wgk_sb])
        QS = float(32 ** -0.5)

        def gla_chain(d):
            sfx = f"_l{d}"
            PA_ = PSA([4 * d + i for i in range(4)])
            lrt_l = [AR.alloc([64, 128], F32, f"lrt{i}" + sfx) for i in range(2)]
            lqk_l = [AR.alloc([128, 2, 128], BF16, f"lqk{i}" + sfx) for i in range(2)]
            tm_l = [AR.alloc([128, 1024], BF16, f"tml{i}" + sfx) for i in range(2)]
            e1 = AR.alloc([128, 128], F32, "e1" + sfx)
            sp = AR.alloc([128, 128], F32, "sp" + sfx)
            enb = AR.alloc([128, 128], F32, "enb" + sfx)
            ebT = AR.alloc([128, 128], F32, "ebT" + sfx)
            enbT = AR.alloc([128, 128], F32, "enbT" + sfx)
            ebl = AR.alloc([128, 1], F32, "ebl" + sfx)
            qdT = AR.alloc([128, 128], BF16, "qdT" + sfx)
            kdT = AR.alloc([128, 128], BF16, "kdT" + sfx)
            kd = AR.alloc([128, 128], BF16, "kd" + sfx)
            qdX = AR.alloc([128, 4, 128], BF16, "qdX" + sfx)
            ATl = AR.alloc([128, 4, 128], BF16, "ATl" + sfx)
            olt = [AR.alloc([128, 256], F32, f"olt{i}" + sfx) for i in range(2)]
            t1 = AR.alloc([128, 256], F32, "t1" + sfx)
            Sg = AR.alloc([128, 256], F32, "Sg" + sfx)
            Sgb = AR.alloc([128, 256], BF16, "Sgb" + sfx)
            TRI = cf[:, C_TRI0 + 128 * d:C_TRI0 + 128 * d + 128]
            MK = cf[:, C_MK0 + 128 * d:C_MK0 + 128 * d + 128]
            P.v("memset", Sg[:], 0.0, writes=[Sg])
            P.v("memset", Sgb[:], 0.0, writes=[Sgb])
            yield
            li = 0
            for t in scan_order(d):
                r0 = t * 128
                lrt = lrt_l[li % 2]
                lqk = lqk_l[li % 2]
                tm = tm_l[li % 2]
                o_ = olt[li % 2]
                li += 1
                P.dma(lrt[:], LRT[:, r0:r0 + 128], writes=[lrt])
                P.dma(lqk[:], LQKT[:, r0:r0 + 128].rearrange("(c p) t -> p c t", p=128), writes=[lqk])
                P.dma(tm[:], TMB[r0:r0 + 128, :], writes=[tm])
                pgk = PA_.nps()
                P.mm(pgk[:, 0:128], lrt[32 * d:32 * d + 16, :], wgk_sb[32 * d:32 * d + 16, :], start=True, stop=False,
                     reads=[lrt, wgk_sb], writes=[pgk])
                P.mm(pgk[:, 0:128], cf[32 * d:32 * d + 1, C_ONES:C_ONES + 128],
                     bcv[32 * d:32 * d + 1, l, B_BGK + 128 * d:B_BGK + 128 * d + 128],
                     start=False, stop=True, reads=[cf, bcv], writes=[pgk])
                yield
                P.act(e1[:], pgk[:, 0:128], AF.Exp, scale=-1.0, reads=[pgk], writes=[e1])
                P.act(sp[:], e1[:], AF.Ln, bias=oneb[:, 0:1], reads=[e1, oneb], writes=[sp])
                pb = PA_.nps()
                P.mm(pb[:, 0:128], TRI, sp[:], reads=[cf, sp], writes=[pb])
                P.mm(pb[:, 128:256], sp[:], TRI, reads=[cf, sp], writes=[pb])
                P.mm(pb[:, 256:257], sp[:], cf[:, C_ONES:C_ONES + 1], reads=[cf, sp], writes=[pb])
                yield
                P.act(ebT[:], pb[:, 128:256], AF.Exp, scale=-1.0 / 16, reads=[pb], writes=[ebT])
                P.act(enbT[:], pb[:, 128:256], AF.Exp, scale=1.0 / 16, reads=[pb], writes=[enbT])
                P.act(enb[:], pb[:, 0:128], AF.Exp, scale=1.0 / 16, reads=[pb], writes=[enb])
                P.act(ebl[:], pb[:, 256:257], AF.Exp, scale=-1.0 / 16, reads=[pb], writes=[ebl])
                P.v("scalar_tensor_tensor", qdT[:], lqk[:, 0, :], QS, ebT[:], ALU.mult, ALU.mult, reads=[lqk, ebT], writes=[qdT])
                P.v("tensor_tensor", kdT[:], lqk[:, 1, :], enbT[:], ALU.mult, reads=[lqk, enbT], writes=[kdT])
                P.v("tensor_tensor", kd[:], tm[:, 384:512], enb[:], ALU.mult, reads=[tm, enb], writes=[kd], eng="pool")
                P.v("tensor_tensor", qdX[:], qdT[:].unsqueeze(1).to_broadcast([128, 4, 128]),
                    cf[:, C_BLK:C_BLK + 4].unsqueeze(2).to_broadcast([128, 4, 128]), ALU.mult, reads=[qdT, cf], writes=[qdX])
                pA = PA_.nps()
                P.mm(pA[:, 0:512], kdT[:], qdX[:].rearrange("p h i -> p (h i)"), reads=[kdT, qdX], writes=[pA])
                pS = PA_.nps()
                P.mm(pS[:, 0:256], kd[:], tm[:, 512:768], reads=[kd, tm], writes=[pS])
                yield
                P.v("tensor_tensor", ATl[:], pA[:, 0:512].rearrange("p (h i) -> p h i", h=4),
                    MK.unsqueeze(1).to_broadcast([128, 4, 128]), ALU.mult, reads=[pA, cf], writes=[ATl])
                po = PA_.nps()
                P.mm(po[:, 0:256], qdT[:], Sgb[:], start=True, stop=False, reads=[qdT, Sgb], writes=[po])
                for h in range(4):
                    P.mm(po[:, h * 64:(h + 1) * 64], ATl[:, h, :], tm[:, 512 + h * 64:512 + (h + 1) * 64], start=False, stop=(h == 3),
                         reads=[ATl, tm], writes=[po])
                P.v("tensor_tensor", t1[:], pS[:, 0:256], cf[:, C_BLKV:C_BLKV + 256], ALU.mult, reads=[pS, cf], writes=[t1])
                P.v("tensor_tensor", t1[:], t1[:], Sg[:], ALU.add, reads=[t1, Sg], writes=[t1])
                P.v("tensor_scalar", Sg[:], t1[:], ebl[:, 0:1], None, ALU.mult, reads=[t1, ebl], writes=[Sg])
                P.v("tensor_copy", Sgb[:], Sg[:], reads=[Sg], writes=[Sgb], eng="pool")
                yield
                P.act(o_[:], po[:, 0:256], AF.Copy, reads=[po], writes=[o_])
                P.dma(LO[d, r0:r0 + 128, :], o_[:], reads=[o_], q="pool")
                yield

        def run_chains(chains):
            if not P.enabled:
                return
            while chains:
                for g_ in list(chains):
                    try:
                        next(g_)
                    except StopIteration:
                        chains.remove(g_)
        run_chains([gdn_chain(0), gdn_chain(1)])
        P.barrier()
        run_chains([gla_chain(0), gla_chain(1)])
        P.barrier()
        if stop_after in ("GDN", "GLA"):
            break

        ph("MERGE")
        AR.reset()
        gol = [AR.alloc([128, 2, 2, 256], F32, f"gol{i}") for i in range(2)]
        tmm = [AR.alloc([128, 1024], BF16, f"tmm{i}") for i in range(2)]
        og = AR.alloc([128, 2, 256], F32, "og")
        osq = AR.alloc([128, 2, 256], F32, "osq")
        oss = AR.alloc([128, 8], F32, "oss")
        szg = AR.alloc([128, 2, 256], F32, "szg")
        mgb = AR.alloc([128, 512], BF16, "mgb")
        mgT = [AR.alloc([128, 4, 128], BF16, f"mgT{i}") for i in range(2)]
        for t in range(2 if not last else 2, NT) if last else range(NT):
            r0 = t * 128
            go_ = gol[t % 2]
            tm = tmm[t % 2]
            mT = mgT[t % 2]
            P.dma(go_[:, 0], GO[:, r0:r0 + 128, :].rearrange("d p c -> p d c"), writes=[go_])
            P.dma(go_[:, 1], LO[:, r0:r0 + 128, :].rearrange("d p c -> p d c"), writes=[go_])
            P.dma(tm[:], TMB[r0:r0 + 128, :], writes=[tm])
            P.v("tensor_tensor", og[:], go_[:, :, 0, :], go_[:, :, 1, :], ALU.add, reads=[go_], writes=[og])
            P.act(osq[:], og[:], AF.Square, reads=[og], writes=[osq])
            P.v("tensor_reduce", oss[:], osq[:].rearrange("p a (h d) -> p (a h) d", d=64), AX.X, ALU.add, reads=[osq], writes=[oss])
            P.act(oss[:], oss[:], AF.Sqrt, scale=1.0 / 64, bias=epsb[:, 0:1], reads=[oss, epsb], writes=[oss])
            P.v("reciprocal", oss[:], oss[:], reads=[oss], writes=[oss])
            P.v("tensor_tensor", og[:].rearrange("p a (h d) -> p (a h) d", d=64), og[:].rearrange("p a (h d) -> p (a h) d", d=64),
                oss[:].unsqueeze(2).to_broadcast([128, 8, 64]), ALU.mult, reads=[og, oss], writes=[og])
            for a, off in ((0, B_GDNN), (1, B_GLAN)):
                P.v("tensor_tensor", og[:, a, :].rearrange("p (h d) -> p h d", d=64), og[:, a, :].rearrange("p (h d) -> p h d", d=64),
                    bcv[:, l, off:off + 64].unsqueeze(1).to_broadcast([128, 4, 64]), ALU.mult, reads=[og, bcv], writes=[og], eng="pool")
            P.act(szg[:, 0, :], tm[:, 0:256], AF.Silu, reads=[tm], writes=[szg])
            P.act(szg[:, 1, :], tm[:, 768:1024], AF.Silu, reads=[tm], writes=[szg])
            P.v("tensor_tensor", mgb[:].rearrange("p (a c) -> p a c", a=2), og[:], szg[:], ALU.mult, reads=[og, szg], writes=[mgb])
            pt = nps()
            ptb = pt.ap.bitcast(BF16)
            for c in range(4):
                P.tr(ptb[:, c * 128:(c + 1) * 128], mgb[:, c * 128:(c + 1) * 128], identb[:], reads=[mgb, identb], writes=[pt])
            P.act(mT[:], ptb[:, 0:512].rearrange("p (c t) -> p c t", c=4), AF.Copy, reads=[pt], writes=[mT])
            P.dma(MT[512:1024, r0:r0 + 128].rearrange("(c p) t -> p c t", p=128), mT[:], reads=[mT], q="pool")
        P.barrier()
        if stop_after == "MERGE":
            break
        def post_res(ys, xs, xo, n, gi, mi, dst, rstd, sq, alloc=None):
            rms_rstd(ys, 8, n, 1.0 / D, rstd, sq, alloc)
            P.v("tensor_tensor", sq[:, :, 0:n], ys[:, :, 0:n], rstd[:, 0:n].unsqueeze(1).to_broadcast([128, 8, n]), ALU.mult,
                reads=[ys, rstd], writes=[sq])
            for k in range(8):
                P.v("scalar_tensor_tensor", xs[:, k, xo:xo + n], sq[:, k, 0:n], drv[:, gi, k, mi:mi + 1], xs[:, k, xo:xo + n],
                    ALU.mult, ALU.add, reads=[sq, drv, xs], writes=[xs])
            P.dma(dst, xs[:, :, xo:xo + n], reads=[xs], q="pool")

        ph("OUT")
        AR.reset()
        WOb = AR.alloc([128, 8, 1024], BF16, "WOb")
        stage = [AR.alloc([128, 1024], F32, f"ostg{i}") for i in range(2)]
        cast_weight(WOb, [WOb[:, k, :] for k in range(8)], [wout_d[l, k * 128:(k + 1) * 128, :] for k in range(8)], stage)
        mts = [AR.alloc([128, 8, 512], BF16, f"mts{i}") for i in range(2)]
        xs2 = [AR.alloc([128, 8, 512], F32, f"xs2{i}") for i in range(2)]
        ys = AR.alloc([128, 8, 512], F32, "ys")
        sq = AR.alloc([128, 8, 512], F32, "sq")
        rstd = AR.alloc([128, 512], F32, "rstd")
        ci = 0
        for seg in ((1,) if last else (0, 1)):
            mi = 1 - seg
            for (c0, n) in chunks(seg):
                m_ = mts[ci % 2]
                x_ = xs2[ci % 2]
                ci += 1
                P.dma(m_[:, :, 0:n], MT[:, c0:c0 + n].rearrange("(k p) t -> p k t", p=128), writes=[m_])
                P.dma(x_[:, :, 0:n], XT[:, c0:c0 + n].rearrange("(k p) t -> p k t", p=128), writes=[x_])
                for c in range(8):
                    ps = nps()
                    for k in range(8):
                        P.mm(ps[:, 0:n], WOb[:, k, c * 128:(c + 1) * 128], m_[:, k, 0:n], start=(k == 0), stop=(k == 7),
                             reads=[WOb, m_], writes=[ps])
                    if c % 2 == 0:
                        P.act(ys[:, c, 0:n], ps[:, 0:n], AF.Copy, reads=[ps], writes=[ys])
                    else:
                        P.v("tensor_copy", ys[:, c, 0:n], ps[:, 0:n], reads=[ps], writes=[ys])
                post_res(ys, x_, 0, n, 1, mi, XM[:, c0:c0 + n].rearrange("(k p) t -> p k t", p=128), rstd, sq)
        P.barrier()
        if stop_after == "OUT":
            break

        ph("FFN")
        AR.reset()
        FW1b = AR.alloc([128, 8, 5120], BF16, "FW1b")
        FW2b = AR.alloc([128, 20, 1024], BF16, "FW2b")
        stage = [AR.alloc([128, 1280], F32, "fstg")]
        cast_weight(FW1b, [FW1b[:, k, hf * 1280:(hf + 1) * 1280] for k in range(8) for hf in range(4)],
                    [fw1_d[l, k * 128:(k + 1) * 128, hf * 1280:(hf + 1) * 1280] for k in range(8) for hf in range(4)], stage)
        cast_weight(FW2b, [FW2b[:, f, :] for f in range(20)], [fw2_d[l, f * 128:(f + 1) * 128, :] for f in range(20)],
                    [Tl(stage[0].ap[:, 0:1024], stage[0].key)])
        WN = 256
        xs = AR.alloc([128, 8, WN], F32, "fxs")
        sq = AR.alloc([128, 8, WN], F32, "fsq")
        hb = AR.alloc([128, 8, WN], BF16, "fhb")
        rstd = AR.alloc([128, WN], F32, "frstd")
        Gt_l = [AR.alloc([128, WN], BF16, f"fG{i}") for i in range(3)]
        FPA = PSA([4, 5, 6, 7])
        zt = AR.alloc([128, 512], BF16, "fzero")
        P.v("memset", zt[:], 0.0, writes=[zt])
        ys = AR.alloc([128, 8, WN], F32, "fys")
        ua = [AR.alloc([128, WN], F32, f"ua{i}") for i in range(2)]
        ug = [AR.alloc([128, WN], F32, f"ug{i}") for i in range(2)]
        za_l = [AR.alloc([128, WN], F32, f"za{i}") for i in range(2)]
        zg_l = [AR.alloc([128, WN], F32, f"zg{i}") for i in range(2)]
        sg_l = [AR.alloc([128, WN], F32, f"sg{i}") for i in range(2)]
        fi = 0
        for seg in ((1,) if last else (0, 1)):
            mi = 1 - seg
            s0, ln = SEGS[seg]
            for w0 in range(s0, s0 + ln, WN - 2):
                nw = min(WN - 2, s0 + ln - w0)
                ni = nw + 2
                lo = max(w0 - 1, s0)
                hi = min(w0 + nw + 1, s0 + ln)
                ledge = (lo > w0 - 1)
                redge = (hi < w0 + nw + 1)
                P.dma(xs[:, :, lo - (w0 - 1):hi - (w0 - 1)], XM[:, lo:hi].rearrange("(k p) t -> p k t", p=128), writes=[xs])
                if ledge:
                    P.v("memset", xs[:, :, 0:1], 0.0, writes=[xs])
                if redge:
                    P.v("memset", xs[:, :, ni - 1:ni], 0.0, writes=[xs])
                rms_rstd(xs, 8, ni, 1.0 / D, rstd, sq, FPA)
                P.v("tensor_tensor", sq[:, :, 0:ni], xs[:, :, 0:ni], rstd[:, 0:ni].unsqueeze(1).to_broadcast([128, 8, ni]),
                    ALU.mult, reads=[xs, rstd], writes=[sq])
                for k in range(8):
                    P.act(hb[:, k, 0:ni], sq[:, k, 0:ni], AF.Identity, scale=drv[:, 2, k, mi:mi + 1],
                          bias=mods[:, l, 24 + k, mi:mi + 1], reads=[sq, drv, mods], writes=[hb])
                def ymm(f_):
                    for c in range(8):
                        yb = c // 2
                        P.mm(pst[:, yb, (c % 2) * 256:(c % 2) * 256 + nw], FW2b[:, f_, c * 128:(c + 1) * 128], Gt_l[f_ % 3][:, 0:nw],
                             start=False, stop=(f_ == 19), reads=[FW2b, Gt_l[f_ % 3]], writes=[PSK[yb]])
                for yb in range(4):
                    P.mm(pst[:, yb, :], zt[:, 0:128], zt[:], start=True, stop=False, reads=[zt], writes=[PSK[yb]])
                for f in range(20):
                    ua_, ug_ = ua[fi % 2], ug[fi % 2]
                    za, zg, sg = za_l[fi % 2], zg_l[fi % 2], sg_l[fi % 2]
                    fi += 1
                    for (fc, dstt) in ((f, ua_), (20 + f, ug_)):
                        ps = FPA.nps()
                        for k in range(8):
                            P.mm(ps[:, 0:ni], FW1b[:, k, fc * 128:(fc + 1) * 128], hb[:, k, 0:ni], start=(k == 0), stop=(k == 7),
                                 reads=[FW1b, hb], writes=[ps])
                        P.act(dstt[:, 0:ni], ps[:, 0:ni], AF.Identity, bias=vec[:, l, V_FBI + fc:V_FBI + fc + 1],
                              reads=[ps, vec], writes=[dstt])
                        if ledge:
                            P.v("memset", dstt[:, 0:1], 0.0, writes=[dstt], eng="pool")
                        if redge:
                            P.v("memset", dstt[:, ni - 1:ni], 0.0, writes=[dstt], eng="pool")
                    for (fc, src, z_) in ((f, ua_, za), (20 + f, ug_, zg)):
                        wv = lambda t: vec[:, l, V_FCW + 40 * t + fc:V_FCW + 40 * t + fc + 1]
                        if fc < 20:
                            P.v("tensor_scalar", z_[:, 0:nw], src[:, 0:nw], wv(0), vec[:, l, V_FCB + fc:V_FCB + fc + 1], ALU.mult, ALU.add,
                                reads=[src, vec], writes=[z_])
                        else:
                            P.act(z_[:, 0:nw], src[:, 0:nw], AF.Identity, scale=wv(0), bias=vec[:, l, V_FCB + fc:V_FCB + fc + 1],
                                  reads=[src, vec], writes=[z_])
                        P.v("scalar_tensor_tensor", z_[:, 0:nw], src[:, 1:nw + 1], wv(1), z_[:, 0:nw], ALU.mult, ALU.add,
                            reads=[src, vec, z_], writes=[z_])
                        P.v("scalar_tensor_tensor", z_[:, 0:nw], src[:, 2:nw + 2], wv(2), z_[:, 0:nw], ALU.mult, ALU.add,
                            reads=[src, vec, z_], writes=[z_])
                    P.act(sg[:, 0:nw], zg[:, 0:nw], AF.Silu, reads=[zg], writes=[sg])
                    Gf = Gt_l[f % 3]
                    P.v("tensor_tensor", Gf[:, 0:nw], za[:, 0:nw], sg[:, 0:nw], ALU.mult, reads=[za, sg], writes=[Gf], eng="pool")
                    if f > 0:
                        ymm(f - 1)
                ymm(19)
                for c in range(8):
                    yb = c // 2
                    src = pst[:, yb, (c % 2) * 256:(c % 2) * 256 + nw]
                    if c % 2 == 0:
                        P.act(ys[:, c, 0:nw], src, AF.Copy, reads=[PSK[yb]], writes=[ys])
                    else:
                        P.v("tensor_copy", ys[:, c, 0:nw], src, reads=[PSK[yb]], writes=[ys])
                if last and seg == 1:
                    dst = out_d[:, w0 - TC:w0 - TC + nw]
                else:
                    dst = XT[:, w0:w0 + nw]
                post_res(ys, xs, 1, nw, 3, mi, dst.rearrange("(k p) t -> p k t", p=128), rstd, sq, FPA)
        P.barrier()
    P.enabled = True
    P.barrier()
    P.finalize(st)
    st.close()
    return nc


_CACHE = {}


def kernel(**inputs):
    inp = {k: np.asarray(v) for k, v in inputs.items()}
    if "nc" not in _CACHE:
        _CACHE["nc"] = build()
    nc = _CACHE["nc"]
    hw = host_weights(inp)
    hc = host_consts()
    maps = []
    for b in range(8):
        m = dict(hw)
        m.update(hc)
        m["xin"] = np.ascontiguousarray(np.concatenate([inp["ctx"][b].T, inp["x"][b].T], axis=1).astype(np.float32))
        m["cc"] = np.ascontiguousarray(np.stack([inp["c"][b], inp["c_ctx"]], axis=1).astype(np.float32))
        maps.append(m)
    res = run_bass_kernel_spmd(nc, maps, core_ids=list(range(8)))
    out = np.stack([np.asarray(res.results[b]["out"]).T for b in range(8)], axis=0)
    return np.ascontiguousarray(out.astype(np.float32))
```
